# Optimizing a Trainium2 kernel written in Bass

```python
import math
import jax, jax.numpy as jnp
from jax import lax
import numpy as np

D_MODEL = 2048
BATCH = 4
SEQ = 4096
DEPTH = 4

N_MIXERS = 3
Q_BLOCK = 128
NEG_INF = -1e30
ROPE_THETA = 10000.0
LN_EPS = 1e-5
RMS_EPS = 1e-6
DEEPNORM_ALPHA = (2 * DEPTH) ** 0.25
DEEPNORM_BETA = (8 * DEPTH) ** -0.25

MLA_HEADS = D_MODEL // 128
MLA_NOPE_DIM = 128
MLA_ROPE_DIM = 64
MLA_V_DIM = 128
MLA_Q_RANK = D_MODEL // 4
MLA_KV_RANK = D_MODEL // 4

DIFF_HEAD_DIM = 128
DIFF_HEADS = D_MODEL // (2 * DIFF_HEAD_DIM)

SB_HEAD_DIM = 128
SB_HEADS = D_MODEL // SB_HEAD_DIM

N_EXPERTS = 32
TOP_K = 4
D_EXPERT = 3 * D_MODEL // 8
SWIGLU_LIMIT = 7.0
SWIGLU_ALPHA = 1.702
MOE_BLOCK = 128

N_MLA_LAYERS = (DEPTH + 2) // 3
N_DIFF_LAYERS = (DEPTH + 1) // 3
N_SB_LAYERS = DEPTH // 3

kernel_name = "hybrid_mla_diff_stickbreak_moe_deepnorm_adaln"


def _layer_norm(x, g, b):
    xf = x.astype(jnp.float32)
    mu = jnp.mean(xf, axis=-1, keepdims=True)
    var = jnp.mean(jnp.square(xf - mu), axis=-1, keepdims=True)
    return ((xf - mu) * lax.rsqrt(var + LN_EPS) * g + b).astype(x.dtype)


def _rms_norm(x, g, eps):
    xf = x.astype(jnp.float32)
    return (xf * lax.rsqrt(jnp.mean(xf * xf, axis=-1, keepdims=True) + eps) * g).astype(x.dtype)


def _rope(t, pos):
    d = t.shape[-1]
    half = d // 2
    inv_freq = ROPE_THETA ** (-jnp.arange(half, dtype=jnp.float32) * (2.0 / d))
    ang = pos.astype(jnp.float32)[:, None] * inv_freq[None, :]
    bshape = (1, pos.shape[0]) + (1,) * (t.ndim - 3) + (half,)
    cos = jnp.cos(ang).reshape(bshape)
    sin = jnp.sin(ang).reshape(bshape)
    tf = t.astype(jnp.float32)
    t1, t2 = tf[..., :half], tf[..., half:]
    return jnp.concatenate([t1 * cos - t2 * sin, t2 * cos + t1 * sin], axis=-1).astype(t.dtype)


def _to_blocks(t):
    b, s = t.shape[:2]
    return jnp.swapaxes(t.reshape((b, s // Q_BLOCK, Q_BLOCK) + t.shape[2:]), 0, 1)


def _from_blocks(t):
    t = jnp.swapaxes(t, 0, 1)
    return t.reshape((t.shape[0], t.shape[1] * t.shape[2]) + t.shape[3:])


def _sweep_query_blocks(block_fn, *q_parts):
    s = q_parts[0].shape[1]
    q_pos = jnp.arange(s, dtype=jnp.int32).reshape(s // Q_BLOCK, Q_BLOCK)
    out = lax.map(lambda args: block_fn(*args), (q_pos,) + tuple(_to_blocks(t) for t in q_parts))
    return _from_blocks(out)


def _mla(h, pos, w_in, q_norm_g, kv_norm_g, w_uq, w_ukv, w_o):
    b, s, _ = h.shape
    lat = h @ w_in
    c_q = _rms_norm(lat[..., :MLA_Q_RANK], q_norm_g, RMS_EPS)
    c_kv = _rms_norm(lat[..., MLA_Q_RANK:MLA_Q_RANK + MLA_KV_RANK], kv_norm_g, RMS_EPS)
    k_rope = _rope(lat[..., MLA_Q_RANK + MLA_KV_RANK:], pos)
    q = (c_q @ w_uq).reshape(b, s, MLA_HEADS, MLA_NOPE_DIM + MLA_ROPE_DIM)
    q_nope = q[..., :MLA_NOPE_DIM]
    q_rope = _rope(q[..., MLA_NOPE_DIM:], pos)
    kv = (c_kv @ w_ukv).reshape(b, s, MLA_HEADS, MLA_NOPE_DIM + MLA_V_DIM)
    k_nope = kv[..., :MLA_NOPE_DIM]
    v = kv[..., MLA_NOPE_DIM:]
    scale = (MLA_NOPE_DIM + MLA_ROPE_DIM) ** -0.5
    k_pos = jnp.arange(s, dtype=jnp.int32)

    def block(q_pos, qn, qr):
        sc = (jnp.einsum('bqhd,bkhd->bhqk', qn, k_nope)
              + jnp.einsum('bqhr,bkr->bhqk', qr, k_rope)).astype(jnp.float32) * scale
        sc = jnp.where(k_pos[None, :] <= q_pos[:, None], sc, NEG_INF)
        p = jax.nn.softmax(sc, axis=-1).astype(v.dtype)
        return jnp.einsum('bhqk,bkhd->bqhd', p, v)

    o = _sweep_query_blocks(block, q_nope, q_rope)
    return o.reshape(b, s, MLA_HEADS * MLA_V_DIM) @ w_o


def _diff_attention(h, pos, layer_idx, w_qkv, lam, subln_g, w_o):
    b, s, _ = h.shape
    nh, hd = DIFF_HEADS, DIFF_HEAD_DIM
    width = nh * 2 * hd
    qkv = h @ w_qkv
    q = _rope(qkv[..., :width].reshape(b, s, nh, 2, hd), pos)
    k = _rope(qkv[..., width:2 * width].reshape(b, s, nh, 2, hd), pos)
    v = qkv[..., 2 * width:].reshape(b, s, nh, 2 * hd)
    lam_init = 0.8 - 0.6 * math.exp(-0.3 * layer_idx)
    lf = lam.astype(jnp.float32)
    lam_full = jnp.exp(jnp.sum(lf[0] * lf[1])) - jnp.exp(jnp.sum(lf[2] * lf[3])) + lam_init
    k1, k2 = k[..., 0, :], k[..., 1, :]
    scale = hd ** -0.5
    k_pos = jnp.arange(s, dtype=jnp.int32)

    def block(q_pos, q1, q2):
        mask = k_pos[None, :] <= q_pos[:, None]
        s1 = jnp.where(mask, jnp.einsum('bqhd,bkhd->bhqk', q1, k1).astype(jnp.float32) * scale, NEG_INF)
        s2 = jnp.where(mask, jnp.einsum('bqhd,bkhd->bhqk', q2, k2).astype(jnp.float32) * scale, NEG_INF)
        a = jax.nn.softmax(s1, axis=-1) - lam_full * jax.nn.softmax(s2, axis=-1)
        return jnp.einsum('bhqk,bkhd->bqhd', a.astype(v.dtype), v)

    o = _sweep_query_blocks(block, q[..., 0, :], q[..., 1, :])
    o = _rms_norm(o, subln_g, LN_EPS) * (1.0 - lam_init)
    return o.reshape(b, s, width) @ w_o


def _stick_breaking(h, w_qkv, w_o):
    b, s, _ = h.shape
    width = SB_HEADS * SB_HEAD_DIM
    qkv = h @ w_qkv
    q = qkv[..., :width].reshape(b, s, SB_HEADS, SB_HEAD_DIM)
    k = qkv[..., width:2 * width].reshape(b, s, SB_HEADS, SB_HEAD_DIM)
    v = qkv[..., 2 * width:].reshape(b, s, SB_HEADS, SB_HEAD_DIM)
    scale = SB_HEAD_DIM ** -0.5
    k_pos = jnp.arange(s, dtype=jnp.int32)

    def block(q_pos, qb):
        z = jnp.einsum('bqhd,bkhd->bhqk', qb, k).astype(jnp.float32) * scale
        strict = k_pos[None, :] < q_pos[:, None]
        log_keep = jnp.where(strict, jax.nn.log_sigmoid(-z), 0.0)
        later = lax.cumsum(log_keep, axis=3, reverse=True) - log_keep
        a = jnp.where(strict, jnp.exp(jax.nn.log_sigmoid(z) + later), 0.0)
        return jnp.einsum('bhqk,bkhd->bqhd', a.astype(v.dtype), v)

    o = _sweep_query_blocks(block, q)
    return o.reshape(b, s, width) @ w_o


def _moe(h, router_w, router_b, w_gate_up, b_gate_up, w_down, b_down):
    b, s, d = h.shape
    t = h.reshape(b * s, d)
    n_tok = b * s
    n_assign = n_tok * TOP_K
    logits = (t @ router_w + router_b).astype(jnp.float32)
    top_logit, top_idx = lax.top_k(logits, TOP_K)
    gates = jax.nn.softmax(top_logit, axis=-1)
    flat_e = top_idx.reshape(-1)
    order = jnp.argsort(flat_e)
    sorted_e = flat_e[order]
    sorted_tok = order // TOP_K
    counts = jnp.bincount(flat_e, length=N_EXPERTS)
    padded = (counts + MOE_BLOCK - 1) // MOE_BLOCK * MOE_BLOCK
    pad_end = jnp.cumsum(padded)
    pad_start = pad_end - padded
    start = jnp.cumsum(counts) - counts
    dest = pad_start[sorted_e] + jnp.arange(n_assign, dtype=jnp.int32) - start[sorted_e]
    n_blocks = -(-n_assign // MOE_BLOCK) + N_EXPERTS
    n_slots = n_blocks * MOE_BLOCK
    slot_tok = jnp.full((n_slots,), n_tok, dtype=jnp.int32).at[dest].set(sorted_tok.astype(jnp.int32))
    block_start = jnp.arange(n_blocks, dtype=jnp.int32) * MOE_BLOCK
    block_expert = jnp.minimum(jnp.searchsorted(pad_end, block_start, side='right'), N_EXPERTS - 1)
    t_pad = jnp.concatenate([t, jnp.zeros((1, d), t.dtype)], axis=0)
    xs = t_pad[slot_tok].reshape(n_blocks, MOE_BLOCK, d)

    def expert_block(args):
        xb, e = args
        gu = xb @ w_gate_up[e] + b_gate_up[e]
        g = jnp.minimum(gu[:, :D_EXPERT], SWIGLU_LIMIT)
        lin = jnp.clip(gu[:, D_EXPERT:], -SWIGLU_LIMIT, SWIGLU_LIMIT)
        act = g * jax.nn.sigmoid(SWIGLU_ALPHA * g) * (lin + 1.0)
        return act @ w_down[e] + b_down[e]

    ys = lax.map(expert_block, (xs, block_expert)).reshape(n_slots, d)
    y_assign = ys[dest] * gates.reshape(-1)[order].astype(h.dtype)[:, None]
    out = jax.ops.segment_sum(y_assign, sorted_tok, num_segments=n_tok)
    return out.reshape(b, s, d)


def setup_inputs(seed: int = 0) -> dict:
    key = jax.random.key(seed)
    ks = jax.random.split(key, 32)

    def nrm(k, shape, std):
        return jax.random.normal(k, shape, jnp.float32) * std

    d = D_MODEL
    mla_in = MLA_Q_RANK + MLA_KV_RANK + MLA_ROPE_DIM
    gate_offset = jnp.repeat(jnp.array([0.0, 0.0, 1.0, 0.0, 0.0, 1.0], jnp.float32), d)
    return {
        "x": nrm(ks[0], (BATCH, SEQ, d), 1.0),
        "c": nrm(ks[1], (BATCH, d), 1.0),
        "ada_w": nrm(ks[2], (DEPTH, d, 6 * d), 0.1 * d ** -0.5),
        "ada_b": nrm(ks[3], (DEPTH, 6 * d), 0.02) + gate_offset,
        "ln_g": 1.0 + nrm(ks[4], (DEPTH, 2, d), 0.02),
        "ln_b": nrm(ks[5], (DEPTH, 2, d), 0.02),
        "mla_w_in": nrm(ks[6], (N_MLA_LAYERS, d, mla_in), d ** -0.5),
        "mla_q_norm_g": 1.0 + nrm(ks[7], (N_MLA_LAYERS, MLA_Q_RANK), 0.02),
        "mla_kv_norm_g": 1.0 + nrm(ks[8], (N_MLA_LAYERS, MLA_KV_RANK), 0.02),
        "mla_w_uq": nrm(ks[9], (N_MLA_LAYERS, MLA_Q_RANK, MLA_HEADS * (MLA_NOPE_DIM + MLA_ROPE_DIM)), MLA_Q_RANK ** -0.5),
        "mla_w_ukv": nrm(ks[10], (N_MLA_LAYERS, MLA_KV_RANK, MLA_HEADS * (MLA_NOPE_DIM + MLA_V_DIM)), MLA_KV_RANK ** -0.5),
        "mla_w_o": nrm(ks[11], (N_MLA_LAYERS, MLA_HEADS * MLA_V_DIM, d), DEEPNORM_BETA * (MLA_HEADS * MLA_V_DIM) ** -0.5),
        "diff_w_qkv": nrm(ks[12], (N_DIFF_LAYERS, d, 3 * DIFF_HEADS * 2 * DIFF_HEAD_DIM), d ** -0.5),
        "diff_lambda": nrm(ks[13], (N_DIFF_LAYERS, 4, DIFF_HEAD_DIM), 0.1),
        "diff_subln_g": 1.0 + nrm(ks[14], (N_DIFF_LAYERS, 2 * DIFF_HEAD_DIM), 0.02),
        "diff_w_o": nrm(ks[15], (N_DIFF_LAYERS, DIFF_HEADS * 2 * DIFF_HEAD_DIM, d), DEEPNORM_BETA * (DIFF_HEADS * 2 * DIFF_HEAD_DIM) ** -0.5),
        "sb_w_qkv": nrm(ks[16], (N_SB_LAYERS, d, 3 * SB_HEADS * SB_HEAD_DIM), d ** -0.5),
        "sb_w_o": nrm(ks[17], (N_SB_LAYERS, SB_HEADS * SB_HEAD_DIM, d), DEEPNORM_BETA * (SB_HEADS * SB_HEAD_DIM) ** -0.5),
        "moe_router_w": nrm(ks[18], (DEPTH, d, N_EXPERTS), d ** -0.5),
        "moe_router_b": nrm(ks[19], (DEPTH, N_EXPERTS), 0.01),
        "moe_w_gate_up": nrm(ks[20], (DEPTH, N_EXPERTS, d, 2 * D_EXPERT), d ** -0.5),
        "moe_b_gate_up": nrm(ks[21], (DEPTH, N_EXPERTS, 2 * D_EXPERT), 0.01),
        "moe_w_down": nrm(ks[22], (DEPTH, N_EXPERTS, D_EXPERT, d), DEEPNORM_BETA * D_EXPERT ** -0.5),
        "moe_b_down": nrm(ks[23], (DEPTH, N_EXPERTS, d), 0.01),
    }


def reference(x, c, ada_w, ada_b, ln_g, ln_b, mla_w_in, mla_q_norm_g, mla_kv_norm_g, mla_w_uq,
              mla_w_ukv, mla_w_o, diff_w_qkv, diff_lambda, diff_subln_g, diff_w_o, sb_w_qkv, sb_w_o,
              moe_router_w, moe_router_b, moe_w_gate_up, moe_b_gate_up, moe_w_down, moe_b_down):
    pos = jnp.arange(x.shape[1], dtype=jnp.int32)
    cond = jax.nn.silu(c)
    for i in range(DEPTH):
        mod = (cond @ ada_w[i] + ada_b[i])[:, None, :]
        shift_t, scale_t, gate_t, shift_c, scale_c, gate_c = jnp.split(mod, 6, axis=-1)
        h = x * (1.0 + scale_t) + shift_t
        kind, j = i % N_MIXERS, i // N_MIXERS
        if kind == 0:
            y = _mla(h, pos, mla_w_in[j], mla_q_norm_g[j], mla_kv_norm_g[j], mla_w_uq[j], mla_w_ukv[j], mla_w_o[j])
        elif kind == 1:
            y = _diff_attention(h, pos, i, diff_w_qkv[j], diff_lambda[j], diff_subln_g[j], diff_w_o[j])
        else:
            y = _stick_breaking(h, sb_w_qkv[j], sb_w_o[j])
        x = _layer_norm(DEEPNORM_ALPHA * x + gate_t * y, ln_g[i, 0], ln_b[i, 0])
        h = x * (1.0 + scale_c) + shift_c
        y = _moe(h, moe_router_w[i], moe_router_b[i], moe_w_gate_up[i], moe_b_gate_up[i], moe_w_down[i], moe_b_down[i])
        x = _layer_norm(DEEPNORM_ALPHA * x + gate_c * y, ln_g[i, 1], ln_b[i, 1])
    return x
```

```python
import math
import contextlib
import numpy as np
import concourse.bass as bass
import concourse.mybir as mybir
from concourse.bass_utils import run_bass_kernel_spmd

F32 = mybir.dt.float32
BF16 = mybir.dt.bfloat16
I32 = mybir.dt.int32
AF = mybir.ActivationFunctionType
ALU = mybir.AluOpType

SAME_ENGINE_SYNC = True

D = 2048
KC = 16
E = 32
FE = 768
LN_EPS = 1e-5
RMS_EPS = 1e-6
DEPTH = 4
ALPHA = (2 * DEPTH) ** 0.25


class Buf:
    __slots__ = ("w", "r", "name")

    def __init__(self, name=""):
        self.w = None
        self.r = []
        self.name = name


class KB:
    def __init__(self, nc, es):
        self.nc = nc
        self.eng = {"pe": nc.tensor, "act": nc.scalar, "dve": nc.vector,
                    "pool": nc.gpsimd, "sp": nc.sync}
        self.sem = {k: es.enter_context(nc.semaphore("s_" + k)) for k in self.eng}
        self.cnt = {k: 0 for k in self.eng}
        self.seen = {k: {} for k in self.eng}
        self.rings = {}
        self.ring_pos = {}
        for q, n in (("sp", 12), ("pool", 12)):
            self.rings[q] = [[es.enter_context(nc.semaphore("d_%s%d" % (q, i))), 0]
                             for i in range(n)]
            self.ring_pos[q] = 0
        self.ninst = 0

    def _wait(self, e, deps):
        need = {}
        for d in deps:
            if d is None:
                continue
            key, sem, val = d
            if key == e and (e == "pe" or not SAME_ENGINE_SYNC):
                continue
            if self.seen[e].get(key, 0) >= val:
                continue
            if key not in need or need[key][1] < val:
                need[key] = (sem, val)
        for key, (sem, val) in need.items():
            self.eng[e].wait_ge(sem, val)
            self.seen[e][key] = val
            self.ninst += 1

    @staticmethod
    def _deps(reads, writes):
        deps = []
        for b in reads:
            deps.append(b.w)
        for b in writes:
            deps.append(b.w)
            deps.extend(b.r)
        return deps

    @staticmethod
    def _commit(ev, reads, writes):
        for b in reads:
            b.r.append(ev)
            if len(b.r) > 48:
                last = {}
                for x in b.r:
                    if x[0] not in last or last[x[0]][2] < x[2]:
                        last[x[0]] = x
                b.r = list(last.values())
        for b in writes:
            b.w = ev
            b.r = []

    def op(self, e, fn, reads=(), writes=()):
        self._wait(e, self._deps(reads, writes))
        inst = fn(self.eng[e])
        self.cnt[e] += 1
        inst.then_inc(self.sem[e], 1)
        self.ninst += 1
        ev = (e, self.sem[e], self.cnt[e])
        self._commit(ev, reads, writes)
        return ev

    def dma(self, q, out, in_, reads=(), writes=(), indirect=None, **kw):
        self._wait(q, self._deps(reads, writes))
        ring = self.rings[q]
        i = self.ring_pos[q]
        self.ring_pos[q] = (i + 1) % len(ring)
        slot = ring[i]
        if indirect is None:
            inst = self.eng[q].dma_start(out=out, in_=in_, **kw)
        else:
            inst = self.eng[q].indirect_dma_start(out=out, in_=in_, **indirect)
        slot[1] += 16
        inst.then_inc(slot[0], 16)
        self.ninst += 1
        ev = ((q, i), slot[0], slot[1])
        self._commit(ev, reads, writes)
        return ev

    def barrier(self):
        for e in self.eng:
            for k in self.eng:
                if k != e and self.cnt[k] > 0 and self.seen[e].get(k, 0) < self.cnt[k]:
                    self.eng[e].wait_ge(self.sem[k], self.cnt[k])
                    self.seen[e][k] = self.cnt[k]
            for q, ring in self.rings.items():
                for i, (sem, val) in enumerate(ring):
                    if val > 0 and self.seen[e].get((q, i), 0) < val:
                        self.eng[e].wait_ge(sem, val)
                        self.seen[e][(q, i)] = val


def _rope_tables(pos, d):
    half = d // 2
    inv = (10000.0 ** (-np.arange(half, dtype=np.float32) * (2.0 / d))).astype(np.float32)
    ang = pos.astype(np.float32)[None, :] * inv[:, None]
    c = np.cos(ang).astype(np.float32)
    s = np.sin(ang).astype(np.float32)
    return np.concatenate([c, c], 0), np.concatenate([s, s], 0)


def _rot_lhsT(d):
    half = d // 2
    m = np.zeros((d, d), np.float32)
    for j in range(half):
        m[j + half, j] = -1.0
        m[j, j + half] = 1.0
    return m


def make_consts(pos):
    c = {}
    c["ident"] = np.eye(128, dtype=np.float32)
    k = np.arange(128)[:, None]
    m = np.arange(128)[None, :]
    c["triu"] = (k < m).astype(np.float32)
    c["sbtri"] = -(k >= m).astype(np.float32)
    c["rot128"] = _rot_lhsT(128)
    r64 = np.zeros((128, 128), np.float32)
    r64[:64, :64] = _rot_lhsT(64)
    c["rot64"] = r64
    c128, s128 = _rope_tables(pos, 128)
    c64, s64 = _rope_tables(pos, 64)
    c["cos128"] = c128
    c["sin128"] = s128
    c["cos64"] = c64
    c["sin64"] = s64
    s = np.arange(128)[:, None]
    t = np.arange(512)[None, :]
    mi = np.stack([((j * 128 + s) <= t) for j in range(4)], 1).astype(np.float32)
    ms = np.stack([((j * 128 + s) < t) for j in range(4)], 1).astype(np.float32)
    c["mask_incl"] = mi
    c["mask_strict"] = ms
    c["iota32"] = np.tile(np.arange(32, dtype=np.float32)[None, :], (128, 1))
    return c


CONST_SHAPES = {
    "ident": (128, 128), "triu": (128, 128), "sbtri": (128, 128), "rot128": (128, 128),
    "rot64": (128, 128), "mask_incl": (128, 4, 512), "mask_strict": (128, 4, 512),
    "iota32": (128, 32),
}


class Prog:
    def __init__(self, cfg, shapes):
        self.cfg = cfg
        self.T = cfg["T"]
        self.NT = self.T // 128
        self.NB = self.T // 512
        self.C = cfg["C"]
        self.TS = min(cfg.get("TS", 2048), cfg["T"])
        self.NBLK = self.C // 128
        self.layers = cfg["layers"]
        self.debug = cfg.get("debug", False)
        nc = bass.Bass("TRN2", target_bir_lowering=False)
        self.nc = nc
        self.din = {}
        for name, shp in shapes.items():
            self.din[name] = nc.dram_tensor(name, list(shp), F32, kind="ExternalInput").ap()
        T = self.T
        self.out = nc.dram_tensor("y", [T, D], F32, kind="ExternalOutput").ap()
        skind = "ExternalOutput" if self.debug else "Internal"

        def scr(name, shape, dt):
            return nc.dram_tensor(name, list(shape), dt, kind=skind).ap()
        nl = len(self.layers)
        self.pair = cfg.get("pair", False)
        self.modv = scr("modv", [nl, 6 * D], F32)
        self.xa = scr("xa", [T, D], F32)
        self.xb = scr("xb", [T, D], F32)
        self.xs = scr("xs", [E * self.C, D], BF16)
        self.ys = scr("ys", [E * self.C, D], F32)
        if not self.pair:
            self.TA = T
            self.QT = scr("QT", [16, 128, T], BF16)
            self.KT = scr("KT", [16, 128, T], BF16)
            self.QrT = scr("QrT", [16, 64, T], BF16)
            self.KrT = scr("KrT", [64, T], BF16)
            self.V = scr("V", [T, D], BF16)
            self.OT = scr("OT", [D, T], BF16)
        else:
            self.TA = 2 * T

            def loc(name, shape, dt):
                return nc.dram_tensor(name, list(shape), dt)
            self.h_send = {"QT": loc("QT_s", [2048, T], BF16), "KT": loc("KT_s", [2048, T], BF16),
                           "QrT": loc("QrT_s", [1024, T], BF16), "KrT": loc("KrT_s", [64, T], BF16),
                           "V": loc("V_s", [T, D], BF16), "OT": loc("OT_s", [2048, T], BF16)}
            self.h_gath = {"QT": loc("QT_g", [4096, T], BF16), "KT": loc("KT_g", [4096, T], BF16),
                           "QrT": loc("QrT_g", [2048, T], BF16), "KrT": loc("KrT_g", [128, T], BF16),
                           "V": loc("V_g", [2 * T, D], BF16), "OT": loc("OT_g", [4096, T], BF16)}
            self.h_loc = {"QT": loc("QT_l", [2048, T], BF16), "KT": loc("KT_l", [2048, T], BF16),
                          "QrT": loc("QrT_l", [1024, T], BF16), "V": loc("V_l", [2 * T, 1024], BF16),
                          "OT": loc("OT_l", [2048, T], BF16)}
            self.QT = self.h_send["QT"].ap().rearrange("(h d) t -> h d t", d=128)
            self.KT = self.h_send["KT"].ap().rearrange("(h d) t -> h d t", d=128)
            self.QrT = self.h_send["QrT"].ap().rearrange("(h d) t -> h d t", d=64)
            self.KrT = self.h_send["KrT"].ap()
            self.V = self.h_send["V"].ap()
            self.OT_s3 = self.h_send["OT"].ap().rearrange("(a r) t -> a r t", a=2)

    def phase(self):
        return contextlib.ExitStack()

    def sb(self, es, name, shape, dt):
        self._n = getattr(self, "_n", 0) + 1
        t = es.enter_context(self.nc.sbuf_tensor("%s_%d" % (name, self._n), list(shape), dt))
        return t, Buf(name)

    def ps(self, es, name, shape, dt=F32):
        self._n = getattr(self, "_n", 0) + 1
        t = es.enter_context(self.nc.psum_tensor("%s_%d" % (name, self._n), list(shape), dt))
        return t, Buf(name)

    def bcast_load(self, q, dst, dstb, src_row):
        self.kb.dma(q, dst, src_row.partition_broadcast(128), writes=[dstb])

    def build(self):
        nc = self.nc
        with contextlib.ExitStack() as es:
            kb = KB(nc, es)
            self.kb = kb
            self.ident_f, self.b_ident_f = self.sb(es, "ident_f", [128, 128], F32)
            self.ident, self.b_ident = self.sb(es, "ident", [128, 128], BF16)
            self.ones_f, self.b_ones_f = self.sb(es, "ones_f", [128, 128], F32)
            self.ones, self.b_ones = self.sb(es, "ones", [128, 128], BF16)
            kb.dma("sp", self.ident_f[:], self.din["ident"], writes=[self.b_ident_f])
            kb.dma("pool", self.ident[:], self.din["ident"], writes=[self.b_ident])
            kb.op("dve", lambda e: e.memset(self.ones_f[:], 1.0), writes=[self.b_ones_f])
            kb.op("dve", lambda e: e.memset(self.ones[:], 1.0), writes=[self.b_ones])
            self.eps_t, self.b_eps = self.sb(es, "eps_t", [128, 1], F32)
            kb.op("dve", lambda e: e.memset(self.eps_t[:], LN_EPS), writes=[self.b_eps])
            self.slots, self.b_slots = self.sb(es, "slots", [128, self.NT, 4], I32)
            self.gates, self.b_gates = self.sb(es, "gates", [128, self.NT, 4], F32)

            self.bc_reg = nc.gpsimd.to_reg(E * self.C - 1)
            self.cc_sem = es.enter_context(nc.semaphore("cc_sem"))
            self.cc_count = 0
            self.cc_dummy, self.b_cc_dummy = self.sb(es, "cc_dummy", [128, 8], F32)
            if self.pair:
                self.rank = nc.sync.partition_id() % 2
                self.rank_pool = nc.gpsimd.partition_id() % 2
            self.prologue_mod()
            kb.barrier()
            x_src = self.din["x"]
            for pos_l, (gi, kind, li, kj) in enumerate(self.layers):
                last = pos_l == len(self.layers) - 1
                if kind == 0:
                    self.mla_proj(pos_l, li, kj, x_src)
                else:
                    self.qkv_proj(pos_l, li, kj, kind, x_src)
                kb.barrier()
                if self.cfg.get("stop") == "proj":
                    break
                if self.pair:
                    self.exchange(["QT", "KT", "V"] + (["QrT", "KrT"] if kind == 0 else []))
                self.attention(kind, gi, kj)
                kb.barrier()
                if self.pair:
                    self.exchange(["OT"])
                if self.cfg.get("stop") == "attn":
                    break
                self.outproj_ln(pos_l, li, kj, kind, x_src)
                kb.barrier()
                if self.cfg.get("stop") == "ln1":
                    break
                self.router(pos_l, li)
                kb.barrier()
                self.experts(li)
                kb.barrier()
                x_dst = self.out if last else self.xb
                self.combine_ln(pos_l, li, x_dst)
                kb.barrier()
                x_src = self.xb
            kb.barrier()
        return nc

    def exchange(self, names):
        kb, nc = self.kb, self.nc
        g = nc.gpsimd
        groups = [[2 * i, 2 * i + 1] for i in range(self.cfg.get("ncores", 8) // 2)]
        for n in names:
            sv = self.h_send[n].ap()
            gv = self.h_gath[n].ap()
            R = sv.shape[0]
            CH = min(R, 512)
            for c in range(R // CH):
                self.cc_count += 1
                g.collective_compute("AllGather", ALU.bypass, replica_groups=groups,
                                     ins=[sv[c * CH:(c + 1) * CH, :].opt()],
                                     outs=[gv[c * 2 * CH:(c + 1) * 2 * CH, :].opt()]).then_inc(self.cc_sem)
        g.wait_ge(self.cc_sem, self.cc_count)
        kb.op("pool", lambda e: e.memset(self.cc_dummy[:], 0.0), writes=[self.b_cc_dummy])
        kb.barrier()
        for n in names:
            if n == "KrT":
                continue
            gv = self.h_gath[n].ap()
            lv = self.h_loc[n].ap()
            if n in ("QT", "KT"):
                src = gv.rearrange("(b c k m) t -> b c k m t", b=2, c=2, k=2)
                dst = lv.rearrange("(k c m) t -> k c m t", k=2, c=2)
                for rk in range(2):
                    kb.dma("sp", dst[rk], src[bass.ds(self.rank, 1), :, rk, :, :].rearrange("o c m t -> (o c) m t"))
            elif n == "QrT":
                src = gv.rearrange("(c k m) t -> c k m t", c=2, k=2)
                dst = lv.rearrange("(k m) t -> k m t", k=2)
                kb.dma("sp", dst, src[bass.ds(self.rank, 1), :, :, :].rearrange("o k m t -> (o k) m t"))
            elif n == "V":
                nch = self.T // 512
                src = gv.rearrange("(c k m) (b w) -> c k m b w", c=nch, k=2, b=2)
                dst = lv.rearrange("(k c m) w -> k c m w", k=2, c=nch)
                for rk in range(2):
                    kb.dma("pool", dst[rk], src[:, rk, :, bass.ds(self.rank_pool, 1), :].rearrange("c m o w -> c m (o w)"))
            elif n == "OT":
                src = gv.rearrange("(d h k m) t -> d h k m t", d=2, h=2, k=2)
                dst = lv.rearrange("(k h m) t -> k h m t", k=2, h=2)
                for rk in range(2):
                    kb.dma("pool", dst[rk], src[bass.ds(self.rank_pool, 1), :, rk, :, :].rearrange("o h m t -> (o h) m t"))
        kb.barrier()

    def prologue_mod(self):
        kb, nc = self.kb, self.nc
        with self.phase() as es:
            c16, b_c16 = self.sb(es, "c16", [128, KC], F32)
            cond, b_cond = self.sb(es, "cond", [128, KC], F32)
            w = [self.sb(es, "adaw%d" % i, [128, KC, 512], F32) for i in range(2)]
            brow, b_brow = self.sb(es, "brow", [1, 6 * D], F32)
            mrow, b_mrow = self.sb(es, "mrow", [1, 6 * D], F32)
            pm = [self.ps(es, "pm%d" % i, [1, 512]) for i in range(2)]
            kb.dma("sp", c16[:], self.din["c16"], writes=[b_c16])
            kb.op("act", lambda e: e.activation(out=cond[:], in_=c16[:], func=AF.Silu),
                  reads=[b_c16], writes=[b_cond])
            it = 0
            for pos_l, (gi, kind, li, kj) in enumerate(self.layers):
                kb.dma("sp", brow[:], self.din["ada_b"][li:li + 1, :], writes=[b_brow])
                wv = self.din["ada_w"][li].rearrange("(kc p) n -> p kc n", p=128)
                for nb in range(6 * D // 512):
                    wt, b_wt = w[it % 2]
                    pt, b_pt = pm[it % 2]
                    it += 1
                    for h in range(2):
                        kb.dma("sp", wt[:, h * 8:(h + 1) * 8, :],
                               wv[:, h * 8:(h + 1) * 8, nb * 512:(nb + 1) * 512], writes=[b_wt])

                    def mm(e, wt=wt, pt=pt):
                        for kc in range(KC):
                            r = e.matmul(pt[:], lhsT=cond[:, kc:kc + 1], rhs=wt[:, kc, :],
                                         start=(kc == 0), stop=(kc == KC - 1))
                        return r
                    kb.op("pe", mm, reads=[b_wt, b_cond], writes=[b_pt])
                    seg = slice(nb * 512, (nb + 1) * 512)
                    kb.op("dve", lambda e, pt=pt, seg=seg: e.tensor_tensor(
                        out=mrow[:, seg], in0=pt[:], in1=brow[:, seg], op=ALU.add),
                        reads=[b_pt, b_brow], writes=[b_mrow])
                for sgm in (1, 4):
                    kb.op("dve", lambda e, sgm=sgm: e.tensor_scalar_add(
                        mrow[:, sgm * D:(sgm + 1) * D], mrow[:, sgm * D:(sgm + 1) * D], 1.0),
                        reads=[b_mrow], writes=[b_mrow])
                kb.dma("sp", self.modv[pos_l:pos_l + 1, :], mrow[:], reads=[b_mrow])

    def make_hT(self, es_outer, pos_l, x_src, seg_shift, seg_scale, tok0, ntok):
        kb, nc = self.kb, self.nc
        hT, b_hT = self.sb(es_outer, "hT", [128, KC, ntok], BF16)
        b_hT_t = [Buf("hT%d" % t) for t in range(ntok // 128)]
        with self.phase() as es:
            sc, b_sc = self.sb(es, "sc", [128, D], F32)
            sh, b_sh = self.sb(es, "sh", [128, D], F32)
            self.bcast_load("sp", sc[:], b_sc, self.modv[pos_l, seg_scale * D:(seg_scale + 1) * D])
            self.bcast_load("sp", sh[:], b_sh, self.modv[pos_l, seg_shift * D:(seg_shift + 1) * D])
            xt = [self.sb(es, "xt%d" % i, [128, D], F32) for i in range(2)]
            hb = [self.sb(es, "hb%d" % i, [128, D], BF16) for i in range(2)]
            ptr = [self.ps(es, "ptr%d" % i, [128, 8, 128], BF16) for i in range(2)]
            for t in range(ntok // 128):
                x_t, b_x = xt[t % 2]
                h_t, b_h = hb[t % 2]
                kb.dma("sp", x_t[:], x_src[tok0 + t * 128:tok0 + (t + 1) * 128, :], writes=[b_x])
                kb.op("dve", lambda e, x_t=x_t: e.tensor_tensor(out=x_t[:], in0=x_t[:], in1=sc[:], op=ALU.mult),
                      reads=[b_sc], writes=[b_x])
                kb.op("dve", lambda e, x_t=x_t, h_t=h_t: e.tensor_tensor(out=h_t[:], in0=x_t[:], in1=sh[:], op=ALU.add),
                      reads=[b_x, b_sh], writes=[b_h])
                for half in range(2):
                    p_t, b_p = ptr[half]

                    def tr(e, p_t=p_t, h_t=h_t, half=half):
                        for j in range(8):
                            kc = half * 8 + j
                            r = e.transpose(p_t[:, j, :], h_t[:, kc * 128:(kc + 1) * 128], self.ident[:])
                        return r
                    kb.op("pe", tr, reads=[b_h, self.b_ident], writes=[b_p])
                    kb.op("act", lambda e, p_t=p_t, half=half, t=t: e.copy(
                        hT[:, half * 8:(half + 1) * 8, t * 128:(t + 1) * 128], p_t[:]),
                        reads=[b_p], writes=[b_hT_t[t]])
        kb.barrier()
        return hT, b_hT_t

    def rope_fm(self, src_ps, b_src, nd, rot, b_rot, cosb, sinb, b_tab, tmp, dst, b_dst, p_rot, b_prot):
        kb = self.kb
        qb, b_qb = tmp["qb"]
        t1, b_t1 = tmp["t1"]
        kb.op("act", lambda e: e.copy(qb[:nd, :], src_ps), reads=[b_src], writes=[b_qb])
        kb.op("pe", lambda e: e.matmul(p_rot[:nd, :], lhsT=rot[:nd, :nd], rhs=qb[:nd, :], start=True, stop=True),
              reads=[b_qb, b_rot], writes=[b_prot])
        kb.op("dve", lambda e: e.tensor_tensor(out=t1[:nd, :], in0=qb[:nd, :], in1=cosb, op=ALU.mult),
              reads=[b_qb, b_tab], writes=[b_t1])
        kb.op("dve", lambda e: e.tensor_tensor(out=qb[:nd, :], in0=p_rot[:nd, :], in1=sinb, op=ALU.mult),
              reads=[b_prot, b_tab], writes=[b_qb])
        kb.op("dve", lambda e: e.tensor_tensor(out=dst, in0=t1[:nd, :], in1=qb[:nd, :], op=ALU.add),
              reads=[b_t1, b_qb], writes=[b_dst])

    def qkv_proj(self, pos_l, li, kj, kind, x_src):
        kb, nc = self.kb, self.nc
        TS = self.TS
        wname = "diff_w_qkv" if kind == 1 else "sb_w_qkv"
        wv = self.din[wname][kj].rearrange("(kc p) n -> p kc n", p=128)
        rope = kind == 1
        qscale = 1.0 if kind == 1 else 128 ** -0.5
        for sbi in range(self.T // TS):
          tok0 = sbi * TS
          T, NB, NT = TS, TS // 512, TS // 128
          with self.phase() as eo:
            hT, b_hT_t = self.make_hT(eo, pos_l, x_src, 0, 1, tok0, TS)
            with self.phase() as es:
                wg = [self.sb(es, "wg%d" % i, [128, KC, 512], BF16) for i in range(3)]
                stg = [self.sb(es, "stg%d" % i, [128, 4, 512], BF16) for i in range(2)]
                vst = [self.sb(es, "vst%d" % i, [128, 512], BF16) for i in range(2)]
                pq = [self.ps(es, "pq%d" % i, [128, 512]) for i in range(3)]
                p_rot, b_prot = self.ps(es, "prot", [128, 512])
                if rope:
                    rot, b_rot = self.sb(es, "rot", [128, 128], BF16)
                    kb.dma("pool", rot[:], self.din["rot128"], writes=[b_rot])
                    cos, b_cos = self.sb(es, "cos", [128, T], F32)
                    sin, b_sin = self.sb(es, "sin", [128, T], F32)
                    b_tab = Buf("tab")
                    kb.dma("sp", cos[:], self.din["cos128"][:, tok0:tok0 + TS], writes=[b_tab])
                    kb.dma("sp", sin[:], self.din["sin128"][:, tok0:tok0 + TS], writes=[b_tab])
                    tmp = {"qb": self.sb(es, "qb", [128, 512], BF16), "t1": self.sb(es, "t1", [128, 512], F32)}
                ng = 12
                loaded = {}

                def load_group(g):
                    w_t, b_w = wg[g % 3]
                    for h in range(2):
                        kb.dma("pool", w_t[:, h * 8:(h + 1) * 8, :],
                               wv[:, h * 8:(h + 1) * 8, g * 512:(g + 1) * 512], writes=[b_w])
                load_group(0)
                load_group(1)
                it = 0
                for g in range(ng):
                    if g + 2 < ng:
                        load_group(g + 2)
                    w_t, b_w = wg[g % 3]
                    if g < 8:
                        dst = self.QT if g < 4 else self.KT
                        for b in range(NB):
                            s_t, b_s = stg[(g * NB + b) % 2]
                            for j in range(4):
                                p_t, b_p = pq[it % 3]
                                it += 1

                                def mm(e, p_t=p_t, w_t=w_t, j=j, b=b):
                                    for kc in range(KC):
                                        r = e.matmul(p_t[:], lhsT=w_t[:, kc, j * 128:(j + 1) * 128],
                                                     rhs=hT[:, kc, b * 512:(b + 1) * 512],
                                                     start=(kc == 0), stop=(kc == KC - 1))
                                    return r
                                kb.op("pe", mm, reads=[b_w] + b_hT_t[b * 4:(b + 1) * 4], writes=[b_p])
                                if rope:
                                    self.rope_fm(p_t[:], b_p, 128, rot, b_rot,
                                                 cos[:, b * 512:(b + 1) * 512], sin[:, b * 512:(b + 1) * 512],
                                                 b_tab, tmp, s_t[:, j, :], b_s, p_rot, b_prot)
                                else:
                                    sc = qscale if g < 4 else 1.0
                                    kb.op("act", lambda e, p_t=p_t, s_t=s_t, j=j, sc=sc: e.activation(
                                        out=s_t[:, j, :], in_=p_t[:], func=AF.Copy, scale=sc),
                                        reads=[b_p], writes=[b_s])
                            hh = (g % 4) * 4
                            kb.dma("sp", dst[hh:hh + 4, :, tok0 + b * 512:tok0 + (b + 1) * 512].rearrange("h d t -> d h t"),
                                   s_t[:], reads=[b_s])
                    else:
                        gv = g - 8
                        for t in range(NT):
                            p_t, b_p = pq[it % 3]
                            it += 1
                            v_t, b_v = vst[t % 2]

                            def mm(e, p_t=p_t, w_t=w_t, t=t):
                                for kc in range(KC):
                                    r = e.matmul(p_t[:], lhsT=hT[:, kc, t * 128:(t + 1) * 128],
                                                 rhs=w_t[:, kc, :], start=(kc == 0), stop=(kc == KC - 1))
                                return r
                            kb.op("pe", mm, reads=[b_w, b_hT_t[t]], writes=[b_p])
                            kb.op("act", lambda e, p_t=p_t, v_t=v_t: e.copy(v_t[:], p_t[:]),
                                  reads=[b_p], writes=[b_v])
                            kb.dma("sp", self.V[tok0 + t * 128:tok0 + (t + 1) * 128, gv * 512:(gv + 1) * 512], v_t[:], reads=[b_v])
          kb.barrier()

    def mla_proj(self, pos_l, li, kj, x_src):
        kb, nc = self.kb, self.nc
        TS = self.TS
        for sbi in range(self.T // TS):
          tok0 = sbi * TS
          T, NB, NT = TS, TS // 512, TS // 128
          with self.phase() as eo:
            cqT, b_cqT = self.sb(eo, "cqT", [128, 4, T], BF16)
            ckvT, b_ckvT = self.sb(eo, "ckvT", [128, 4, T], BF16)
            krT, b_krT = self.sb(eo, "krT", [64, T], BF16)
            b_cq_t = [Buf("cq%d" % t) for t in range(NT)]
            with self.phase() as e1:
                hT, b_hT_t = self.make_hT(e1, pos_l, x_src, 0, 1, tok0, TS)
                win, b_win = self.sb(e1, "win", [128, KC, 1088], BF16)
                wv = self.din["mla_w_in"][kj].rearrange("(kc p) n -> p kc n", p=128)
                for h in range(4):
                    kb.dma("pool", win[:, h * 4:(h + 1) * 4, :], wv[:, h * 4:(h + 1) * 4, :], writes=[b_win])
                gq, b_gq = self.sb(e1, "gq", [128, 512], F32)
                gkv, b_gkv = self.sb(e1, "gkv", [128, 512], F32)
                self.bcast_load("sp", gq[:], b_gq, self.din["mla_q_norm_g"][kj])
                self.bcast_load("sp", gkv[:], b_gkv, self.din["mla_kv_norm_g"][kj])
                plat = [self.ps(e1, "plat%d" % i, [128, 512]) for i in range(3)]
                ptr2, b_ptr2 = self.ps(e1, "ptr2", [128, 9, 128], BF16)
                junk, b_junk = self.sb(e1, "junk", [128, 512], BF16)
                st, b_st = self.sb(e1, "st", [128, 8], F32)
                cb = [self.sb(e1, "cb%d" % i, [128, 1088], BF16) for i in range(2)]
                for t in range(NT):
                    c_t, b_c = cb[t % 2]
                    for ci, (c0, cn) in enumerate(((0, 512), (512, 512), (1024, 64))):
                        p_t, b_p = plat[ci]

                        def mm(e, p_t=p_t, c0=c0, cn=cn, t=t):
                            for kc in range(KC):
                                r = e.matmul(p_t[:, :cn], lhsT=hT[:, kc, t * 128:(t + 1) * 128],
                                             rhs=win[:, kc, c0:c0 + cn], start=(kc == 0), stop=(kc == KC - 1))
                            return r
                        kb.op("pe", mm, reads=[b_win, b_hT_t[t]], writes=[b_p])
                        if ci < 2:
                            g_t, b_g = (gq, b_gq) if ci == 0 else (gkv, b_gkv)
                            kb.op("act", lambda e, p_t=p_t, ci=ci: e.activation(
                                out=junk[:], in_=p_t[:], func=AF.Square, accum_out=st[:, ci:ci + 1]),
                                reads=[b_p], writes=[b_junk, b_st])
                            kb.op("act", lambda e, ci=ci: e.activation(
                                out=st[:, 2 + ci:3 + ci], in_=st[:, ci:ci + 1], func=AF.Sqrt,
                                bias=RMS_EPS, scale=1.0 / 512), reads=[b_st], writes=[b_st])
                            kb.op("dve", lambda e, ci=ci: e.reciprocal(st[:, 4 + ci:5 + ci], st[:, 2 + ci:3 + ci]),
                                  reads=[b_st], writes=[b_st])
                            kb.op("dve", lambda e, p_t=p_t, ci=ci, c_t=c_t, g_t=g_t, c0=c0: e.scalar_tensor_tensor(
                                out=c_t[:, c0:c0 + 512], in0=p_t[:], scalar=st[:, 4 + ci:5 + ci], in1=g_t[:],
                                op0=ALU.mult, op1=ALU.mult), reads=[b_p, b_st, b_g], writes=[b_c])
                        else:
                            kb.op("act", lambda e, p_t=p_t, c_t=c_t: e.copy(c_t[:, 1024:1088], p_t[:, :64]),
                                  reads=[b_p], writes=[b_c])

                    def tr(e, c_t=c_t):
                        for j in range(8):
                            r = e.transpose(ptr2[:, j, :], c_t[:, j * 128:(j + 1) * 128], self.ident[:])
                        r = e.transpose(ptr2[:64, 8, :], c_t[:, 1024:1088], self.ident[:])
                        return r
                    kb.op("pe", tr, reads=[b_c, self.b_ident], writes=[b_ptr2])
                    sl = slice(t * 128, (t + 1) * 128)
                    kb.op("act", lambda e, sl=sl: e.copy(cqT[:, :, sl], ptr2[:, 0:4, :]),
                          reads=[b_ptr2], writes=[b_cq_t[t]])
                    kb.op("dve", lambda e, sl=sl: e.tensor_copy(ckvT[:, :, sl], ptr2[:, 4:8, :]),
                          reads=[b_ptr2], writes=[b_cq_t[t]])
                    kb.op("act", lambda e, sl=sl: e.copy(krT[:, sl], ptr2[:64, 8, :]),
                          reads=[b_ptr2], writes=[b_cq_t[t]])
            kb.barrier()
            with self.phase() as e2:
                wuq, b_wuq = self.sb(e2, "wuq", [128, 4, 3072], BF16)
                wukv, b_wukv = self.sb(e2, "wukv", [128, 4, 4096], BF16)
                wq_v = self.din["mla_w_uq"][kj].rearrange("(kc p) n -> p kc n", p=128)
                wk_v = self.din["mla_w_ukv"][kj].rearrange("(kc p) n -> p kc n", p=128)
                for c0 in range(0, 3072, 1024):
                    kb.dma("pool", wuq[:, :, c0:c0 + 1024], wq_v[:, :, c0:c0 + 1024], writes=[b_wuq])
                for c0 in range(0, 4096, 1024):
                    kb.dma("pool", wukv[:, :, c0:c0 + 1024], wk_v[:, :, c0:c0 + 1024], writes=[b_wukv])
                rot, b_rot = self.sb(e2, "rot", [128, 128], BF16)
                kb.dma("pool", rot[:], self.din["rot64"], writes=[b_rot])
                cos, b_cos = self.sb(e2, "cos", [64, T], F32)
                sin, b_sin = self.sb(e2, "sin", [64, T], F32)
                b_tab = Buf("tab")
                kb.dma("sp", cos[:], self.din["cos64"][0:64, tok0:tok0 + TS], writes=[b_tab])
                kb.dma("sp", sin[:], self.din["sin64"][0:64, tok0:tok0 + TS], writes=[b_tab])
                tmp = {"qb": self.sb(e2, "qb", [128, 512], BF16), "t1": self.sb(e2, "t1", [128, 512], F32)}
                qn_st, b_qn = self.sb(e2, "qn_st", [128, 16, 512], BF16)
                kn_st, b_kn = self.sb(e2, "kn_st", [128, 16, 512], BF16)
                qr_st, b_qr = self.sb(e2, "qr_st", [64, 16, 512], BF16)
                kr_st, b_kr = self.sb(e2, "kr_st", [64, 512], BF16)
                vst = [self.sb(e2, "vst%d" % i, [128, D], BF16) for i in range(2)]
                pq = [self.ps(e2, "pq%d" % i, [128, 512]) for i in range(3)]
                p_rot, b_prot = self.ps(e2, "prot", [128, 512])
                pv, b_pv = self.ps(e2, "pv", [128, D])
                it = 0
                wukv4 = wukv[:].rearrange("p k (h c) -> p k h c", c=256)
                for b in range(NB):
                    bs = slice(b * 512, (b + 1) * 512)
                    gs = slice(tok0 + b * 512, tok0 + (b + 1) * 512)
                    rd = b_cq_t[b * 4:(b + 1) * 4]
                    p_t, b_p = pq[it % 3]
                    it += 1
                    kb.op("pe", lambda e, p_t=p_t, bs=bs: e.matmul(
                        p_t[:64, :], lhsT=self.ident[:64, :64], rhs=krT[:, bs], start=True, stop=True),
                        reads=rd + [self.b_ident], writes=[b_p])
                    self.rope_fm(p_t[:64, :], b_p, 64, rot, b_rot, cos[:, bs], sin[:, bs], b_tab, tmp,
                                 kr_st[:, :], b_kr, p_rot, b_prot)
                    kb.dma("sp", self.KrT[:, gs], kr_st[:], reads=[b_kr])
                    for h in range(16):
                        p_t, b_p = pq[it % 3]
                        it += 1

                        def mm(e, p_t=p_t, h=h, bs=bs):
                            for kc in range(4):
                                r = e.matmul(p_t[:], lhsT=wuq[:, kc, h * 192:h * 192 + 128], rhs=cqT[:, kc, bs],
                                             start=(kc == 0), stop=(kc == 3))
                            return r
                        kb.op("pe", mm, reads=rd + [b_wuq], writes=[b_p])
                        kb.op("act", lambda e, p_t=p_t, h=h: e.copy(qn_st[:, h, :], p_t[:]),
                              reads=[b_p], writes=[b_qn])
                        p_t, b_p = pq[it % 3]
                        it += 1

                        def mm(e, p_t=p_t, h=h, bs=bs):
                            for kc in range(4):
                                r = e.matmul(p_t[:64, :], lhsT=wuq[:, kc, h * 192 + 128:h * 192 + 192],
                                             rhs=cqT[:, kc, bs], start=(kc == 0), stop=(kc == 3))
                            return r
                        kb.op("pe", mm, reads=rd + [b_wuq], writes=[b_p])
                        self.rope_fm(p_t[:64, :], b_p, 64, rot, b_rot, cos[:, bs], sin[:, bs], b_tab, tmp,
                                     qr_st[:, h, :], b_qr, p_rot, b_prot)
                        p_t, b_p = pq[it % 3]
                        it += 1

                        def mm(e, p_t=p_t, h=h, bs=bs):
                            for kc in range(4):
                                r = e.matmul(p_t[:], lhsT=wukv[:, kc, h * 256:h * 256 + 128], rhs=ckvT[:, kc, bs],
                                             start=(kc == 0), stop=(kc == 3))
                            return r
                        kb.op("pe", mm, reads=rd + [b_wukv], writes=[b_p])
                        kb.op("dve", lambda e, p_t=p_t, h=h: e.tensor_copy(kn_st[:, h, :], p_t[:]),
                              reads=[b_p], writes=[b_kn])
                    kb.dma("sp", self.QT[:, :, gs].rearrange("h d t -> d h t"), qn_st[:], reads=[b_qn])
                    kb.dma("sp", self.KT[:, :, gs].rearrange("h d t -> d h t"), kn_st[:], reads=[b_kn])
                    kb.dma("sp", self.QrT[:, :, gs].rearrange("h d t -> d h t"), qr_st[:], reads=[b_qr])
                    for tt in range(4):
                        t = b * 4 + tt
                        v_t, b_v = vst[t % 2]

                        def mm(e, t=t):
                            for n in range(4):
                                for kc in range(4):
                                    r = e.matmul(pv[:, n * 512:(n + 1) * 512],
                                                 lhsT=ckvT[:, kc, t * 128:(t + 1) * 128],
                                                 rhs=wukv4[:, kc, n * 4:(n + 1) * 4, 128:256],
                                                 start=(kc == 0), stop=(kc == 3))
                            return r
                        kb.op("pe", mm, reads=rd + [b_wukv], writes=[b_pv])
                        kb.op("act", lambda e, v_t=v_t: e.copy(v_t[:], pv[:]), reads=[b_pv], writes=[b_v])
                        kb.dma("sp", self.V[tok0 + t * 128:tok0 + (t + 1) * 128, :], v_t[:], reads=[b_v])
          kb.barrier()

    def attention(self, kind, gi, kj):
        kb, nc = self.kb, self.nc
        T, NB, NT = self.TA, self.TA // 512, self.TA // 128
        TO = self.T
        nheads = 8 if kind == 1 else 16
        if self.pair:
            nheads //= 2
        with self.phase() as es:
            mask, b_mask = self.sb(es, "mask", [128, 4, 512], BF16)
            kb.dma("pool", mask[:], self.din["mask_strict" if kind == 2 else "mask_incl"], writes=[b_mask])
            nsub = 2 if kind == 1 else 1
            ndv = 2 if kind == 1 else 1
            kt_sb = [self.sb(es, "kt%d" % i, [128, nsub, T], BF16) for i in range(2)]
            qt_sb = [self.sb(es, "qt%d" % i, [128, nsub, T], BF16) for i in range(2)]
            v_sb = [self.sb(es, "v%d" % i, [128, NT, ndv * 128], BF16) for i in range(2)]
            pt = [self.sb(es, "pt%d" % i, [128, 512], BF16) for i in range(4)]
            ost = [self.sb(es, "ost%d" % i, [128, ndv, 512], BF16) for i in range(2)]
            rden, b_rden = self.sb(es, "rden", [128, 512], F32)
            pacc = [self.sb(es, "pacc%d" % i, [128, 512], F32) for i in range(2)]
            NS = 3 if kind == 2 else 2
            ps_s = [self.ps(es, "ps_s%d" % i, [128, 512]) for i in range(NS)]
            ps_o = [self.ps(es, "ps_o%d" % i, [128, 512]) for i in range(4 if kind == 1 else 2)]
            ps_d, b_psd = self.ps(es, "ps_d", [128, 512])
            if kind == 0:
                krt, b_krt = self.sb(es, "krt", [64, T], BF16)
                if self.pair:
                    for rk in range(2):
                        kb.dma("sp", krt[:, rk * TO:(rk + 1) * TO], self.h_gath["KrT"].ap()[rk * 64:(rk + 1) * 64, :], writes=[b_krt])
                else:
                    kb.dma("sp", krt[:], self.KrT, writes=[b_krt])
                qrt = [self.sb(es, "qrt%d" % i, [64, T], BF16) for i in range(2)]
                scale = 192 ** -0.5
            elif kind == 1:
                scale = 128 ** -0.5
                lam_init = 0.8 - 0.6 * math.exp(-0.3 * gi)
                lrow, b_lrow = self.sb(es, "lrow", [1, 512], F32)
                lsc, b_lsc = self.sb(es, "lsc", [1, 8], F32)
                ljunk, b_ljunk = self.sb(es, "ljunk", [1, 128], F32)
                nlam, b_nlam = self.sb(es, "nlam", [128, 1], F32)
                kb.dma("sp", lrow[:], self.din["diff_lambda"][kj:kj + 1].rearrange("o a d -> o (a d)"), writes=[b_lrow])
                for i in range(2):
                    kb.op("dve", lambda e, i=i: e.scalar_tensor_tensor(
                        out=ljunk[:], in0=lrow[:, (2 * i) * 128:(2 * i + 1) * 128], scalar=1.0,
                        in1=lrow[:, (2 * i + 1) * 128:(2 * i + 2) * 128], op0=ALU.mult, op1=ALU.mult,
                        accum_out=lsc[:, i:i + 1]), reads=[b_lrow], writes=[b_ljunk, b_lsc])
                kb.op("act", lambda e: e.activation(out=lsc[:, 2:4], in_=lsc[:, 0:2], func=AF.Exp),
                      reads=[b_lsc], writes=[b_lsc])
                kb.op("dve", lambda e: e.tensor_tensor(out=lsc[:, 4:5], in0=lsc[:, 3:4], in1=lsc[:, 2:3], op=ALU.subtract),
                      reads=[b_lsc], writes=[b_lsc])
                kb.op("dve", lambda e: e.tensor_scalar_add(lsc[:, 4:5], lsc[:, 4:5], -lam_init),
                      reads=[b_lsc], writes=[b_lsc])
                kb.op("pe", lambda e: e.matmul(ps_d[:, 0:1], lhsT=self.ones_f[0:1, :], rhs=lsc[:, 4:5], start=True, stop=True),
                      reads=[b_lsc, self.b_ones_f], writes=[b_psd])
                kb.op("dve", lambda e: e.tensor_copy(nlam[:], ps_d[:, 0:1]), reads=[b_psd], writes=[b_nlam])
                gsub, b_gsub = self.sb(es, "gsub", [128, 2], F32)
                kb.dma("sp", gsub[:], self.din["diff_subln_g"][kj].rearrange("(c p) -> p c", p=128), writes=[b_gsub],
                       allow_slow_non_contiguous=True)
                kb.op("dve", lambda e: e.tensor_scalar_mul(gsub[:], gsub[:], 1.0 - lam_init), reads=[b_gsub], writes=[b_gsub])
                o1, b_o1 = self.sb(es, "o1", [128, 2, 512], F32)
                o2, b_o2 = self.sb(es, "o2", [128, 2, 512], F32)
                sq, b_sq = self.sb(es, "sq", [128, 2, 512], F32)
            else:
                scale = 1.0
                sbtri, b_sbtri = self.sb(es, "sbtri", [128, 128], BF16)
                kb.dma("pool", sbtri[:], self.din["sbtri"], writes=[b_sbtri])
                negones, b_negones = self.sb(es, "negones", [128, 128], BF16)
                kb.op("dve", lambda e: e.memset(negones[:], -1.0), writes=[b_negones])
                e1t = [self.sb(es, "e1t%d" % i, [128, 512], F32) for i in range(2)]
                lpt = [self.sb(es, "lpt%d" % i, [128, 512], BF16) for i in range(3)]
                lsum, b_lsum = self.sb(es, "lsum", [128, 512], F32)
                lsbs = [self.sb(es, "lsb%d" % i, [128, 512], BF16) for i in range(2)]

            def load_head_pair(h):
                k_t, b_k = kt_sb[h % 2]
                q_t, b_q = qt_sb[h % 2]
                v_t, b_v = v_sb[h % 2]
                NTO = TO // 128
                ktv = self.h_loc["KT"].ap().rearrange("(a m) t -> a m t", a=2)
                qtv = self.h_loc["QT"].ap().rearrange("(a m) t -> a m t", a=2)
                for s in range(nsub):
                    ul = h * nsub + s
                    for rk in range(2):
                        kb.dma("sp", k_t[:, s, rk * TO:(rk + 1) * TO], ktv[rk, ul * 128:(ul + 1) * 128, :], writes=[b_k])
                        kb.dma("sp", q_t[:, s, rk * TO:(rk + 1) * TO], qtv[rk, ul * 128:(ul + 1) * 128, :], writes=[b_q])
                vw = ndv * 128
                vv = self.h_loc["V"].ap()
                for rk in range(2):
                    kb.dma("sp", v_t[:, rk * NTO:(rk + 1) * NTO, :],
                           vv[rk * TO:(rk + 1) * TO, h * vw:(h + 1) * vw].rearrange("(kt p) d -> p kt d", p=128), writes=[b_v])
                if kind == 0:
                    qrv = self.h_loc["QrT"].ap().rearrange("(a m) t -> a m t", a=2)
                    for rk in range(2):
                        kb.dma("sp", qrt[h % 2][0][:, rk * TO:(rk + 1) * TO], qrv[rk, h * 64:(h + 1) * 64, :],
                               writes=[qrt[h % 2][1]])

            def load_head(h):
                if self.pair:
                    return load_head_pair(h)
                k_t, b_k = kt_sb[h % 2]
                q_t, b_q = qt_sb[h % 2]
                v_t, b_v = v_sb[h % 2]
                for s in range(nsub):
                    u = h * nsub + s
                    kb.dma("sp", k_t[:, s, :], self.KT[u], writes=[b_k])
                    kb.dma("sp", q_t[:, s, :], self.QT[u], writes=[b_q])
                vw = ndv * 128
                kb.dma("sp", v_t[:], self.V[:, h * vw:(h + 1) * vw].rearrange("(kt p) d -> p kt d", p=128), writes=[b_v])
                if kind == 0:
                    kb.dma("sp", qrt[h % 2][0][:], self.QrT[h], writes=[qrt[h % 2][1]])

            steps = []
            for h in range(nheads):
                for qb in range(NB):
                    nkt = 4 * (qb + 1)
                    for s in range(nsub):
                        order = list(range(nkt)) if kind != 2 else list(range(nkt - 1, -1, -1))
                        for oi, kt in enumerate(order):
                            steps.append((h, qb, s, oi, kt, nkt))

            def emit_S(i):
                h, qb, s, oi, kt, nkt = steps[i]
                k_t, b_k = kt_sb[h % 2]
                q_t, b_q = qt_sb[h % 2]
                p_s, b_ps = ps_s[i % NS]
                ks = slice(kt * 128, (kt + 1) * 128)
                qs = slice(qb * 512, (qb + 1) * 512)
                if kind == 0:
                    q_r, b_qr = qrt[h % 2]

                    def mm(e):
                        e.matmul(p_s[:], lhsT=k_t[:, 0, ks], rhs=q_t[:, 0, qs], start=True, stop=False)
                        return e.matmul(p_s[:], lhsT=krt[:, ks], rhs=q_r[:, qs], start=False, stop=True)
                    kb.op("pe", mm, reads=[b_k, b_q, b_krt, b_qr], writes=[b_ps])
                else:
                    kb.op("pe", lambda e: e.matmul(p_s[:], lhsT=k_t[:, s, ks], rhs=q_t[:, s, qs], start=True,
                                                   stop=(kind != 2)), reads=[b_k, b_q], writes=[b_ps])

            def stage_A(i):
                h, qb, s, oi, kt, nkt = steps[i]
                p_s, b_ps = ps_s[i % NS]
                first = oi == 0
                lastk = oi == nkt - 1
                diag = kt >= 4 * qb
                j = kt - 4 * qb
                e_t, b_e = e1t[i % 2]
                l_t, b_l = lpt[i % 3]
                lsb_w, b_lsbw = lsbs[i % 2]
                lsb_r, b_lsbr = lsbs[(i + 1) % 2]
                kb.op("act", lambda e: e.activation(out=e_t[:], in_=p_s[:], func=AF.Exp), reads=[b_ps], writes=[b_e])
                kb.op("act", lambda e: e.activation(out=l_t[:], in_=e_t[:], func=AF.Ln, bias=1.0, scale=1.0),
                      reads=[b_e], writes=[b_l])
                if diag:
                    kb.op("dve", lambda e: e.tensor_tensor(out=l_t[:], in0=l_t[:], in1=mask[:, j, :], op=ALU.mult),
                          reads=[b_mask], writes=[b_l])

                def mm2(e):
                    r = e.matmul(p_s[:], lhsT=sbtri[:], rhs=l_t[:], start=False, stop=first)
                    if not first:
                        r = e.matmul(p_s[:], lhsT=negones[:], rhs=lsb_r[:], start=False, stop=True)
                    return r
                kb.op("pe", mm2, reads=[b_l, b_sbtri, b_negones] + ([] if first else [b_lsbr]), writes=[b_ps])
                if not lastk:
                    if first:
                        kb.op("dve", lambda e: e.tensor_copy(lsum[:], l_t[:]), reads=[b_l], writes=[b_lsum])
                    else:
                        kb.op("dve", lambda e: e.tensor_tensor(out=lsum[:], in0=lsum[:], in1=l_t[:], op=ALU.add),
                              reads=[b_l], writes=[b_lsum])
                    kb.op("dve", lambda e: e.tensor_copy(lsb_w[:], lsum[:]), reads=[b_lsum], writes=[b_lsbw])

            load_head(0)
            emit_S(0)
            if kind == 2:
                emit_S(1)
                stage_A(0)
            grp = 0
            for i, (h, qb, s, oi, kt, nkt) in enumerate(steps):
                if qb == 0 and s == 0 and oi == 0 and h + 1 < nheads:
                    load_head(h + 1)
                if kind == 2:
                    if i + 2 < len(steps):
                        emit_S(i + 2)
                    if i + 1 < len(steps):
                        stage_A(i + 1)
                elif i + 1 < len(steps):
                    emit_S(i + 1)
                v_t, b_v = v_sb[h % 2]
                p_s, b_ps = ps_s[i % NS]
                p_t, b_pt = pt[i % 4]
                qs = slice(qb * 512, (qb + 1) * 512)
                first = oi == 0
                lastk = oi == nkt - 1
                diag = kt >= 4 * qb
                j = kt - 4 * qb
                if first:
                    grp += 1
                pa, b_pa = pacc[grp % 2]
                o_st, b_ost = ost[(h * NB + qb) % 2]
                kb.op("act", lambda e: e.activation(out=p_t[:], in_=p_s[:], func=AF.Exp, scale=scale),
                      reads=[b_ps], writes=[b_pt])
                if diag:
                    kb.op("dve", lambda e: e.tensor_tensor(out=p_t[:], in0=p_t[:], in1=mask[:, j, :], op=ALU.mult),
                          reads=[b_mask], writes=[b_pt])
                if kind != 2:
                    if first:
                        kb.op("dve", lambda e: e.tensor_copy(pa[:], p_t[:]), reads=[b_pt], writes=[b_pa])
                    else:
                        kb.op("dve", lambda e: e.tensor_tensor(out=pa[:], in0=pa[:], in1=p_t[:], op=ALU.add),
                              reads=[b_pt], writes=[b_pa])
                for c in range(ndv):
                    p_o, b_po = ps_o[s * 2 + c] if kind == 1 else ps_o[(h * NB + qb) % 2]
                    kb.op("pe", lambda e, p_o=p_o, c=c: e.matmul(
                        p_o[:], lhsT=v_t[:, kt, c * 128:(c + 1) * 128], rhs=p_t[:], start=first, stop=lastk),
                        reads=[b_v, b_pt], writes=[b_po])
                if not lastk:
                    continue
                if kind == 2:
                    p_o, b_po = ps_o[(h * NB + qb) % 2]
                    kb.op("act", lambda e: e.copy(o_st[:, 0, :], p_o[:]), reads=[b_po], writes=[b_ost])
                else:
                    kb.op("pe", lambda e: e.matmul(ps_d[:], lhsT=self.ones_f[:], rhs=pa[:], start=True, stop=True),
                          reads=[self.b_ones_f, b_pa], writes=[b_psd])
                    kb.op("act", lambda e: e.activation(out=rden[:], in_=ps_d[:], func=AF.Ln), reads=[b_psd], writes=[b_rden])
                    kb.op("act", lambda e: e.activation(out=rden[:], in_=rden[:], func=AF.Exp, scale=-1.0),
                          reads=[b_rden], writes=[b_rden])
                    if kind == 0:
                        p_o, b_po = ps_o[(h * NB + qb) % 2]
                        kb.op("dve", lambda e: e.tensor_tensor(out=o_st[:, 0, :], in0=p_o[:], in1=rden[:], op=ALU.mult),
                              reads=[b_po, b_rden], writes=[b_ost])
                    else:
                        o_t, b_o = (o1, b_o1) if s == 0 else (o2, b_o2)
                        for c in range(2):
                            kb.op("dve", lambda e, c=c: e.tensor_tensor(
                                out=o_t[:, c, :], in0=ps_o[s * 2 + c][0][:], in1=rden[:], op=ALU.mult),
                                reads=[ps_o[s * 2 + c][1], b_rden], writes=[b_o])
                if kind == 1 and s == 0:
                    continue
                if kind == 1:
                    kb.op("dve", lambda e: e.scalar_tensor_tensor(
                        out=o1[:], in0=o2[:], scalar=nlam[:, 0:1], in1=o1[:], op0=ALU.mult, op1=ALU.add),
                        reads=[b_o2, b_nlam], writes=[b_o1])
                    kb.op("act", lambda e: e.activation(out=sq[:], in_=o1[:], func=AF.Square), reads=[b_o1], writes=[b_sq])

                    def mmss(e):
                        e.matmul(ps_d[:], lhsT=self.ones_f[:], rhs=sq[:, 0, :], start=True, stop=False)
                        return e.matmul(ps_d[:], lhsT=self.ones_f[:], rhs=sq[:, 1, :], start=False, stop=True)
                    kb.op("pe", mmss, reads=[b_sq, self.b_ones_f], writes=[b_psd])
                    kb.op("act", lambda e: e.activation(out=rden[:], in_=ps_d[:], func=AF.Ln, bias=self.eps_t[:, 0:1], scale=1.0 / 256),
                          reads=[b_psd, self.b_eps], writes=[b_rden])
                    kb.op("act", lambda e: e.activation(out=rden[:], in_=rden[:], func=AF.Exp, scale=-0.5),
                          reads=[b_rden], writes=[b_rden])
                    for c in range(2):
                        kb.op("dve", lambda e, c=c: e.scalar_tensor_tensor(
                            out=o_st[:, c, :], in0=o1[:, c, :], scalar=gsub[:, c:c + 1], in1=rden[:],
                            op0=ALU.mult, op1=ALU.mult), reads=[b_o1, b_gsub, b_rden], writes=[b_ost])
                r0 = h * ndv * 128
                if self.pair:
                    nbh = TO // 512
                    ts_ = slice((qb % nbh) * 512, (qb % nbh + 1) * 512)
                    kb.dma("sp", self.OT_s3[qb // nbh, r0:r0 + ndv * 128, ts_].rearrange("(c p) t -> p c t", p=128),
                           o_st[:], reads=[b_ost])
                else:
                    kb.dma("sp", self.OT[r0:r0 + ndv * 128, qs].rearrange("(c p) t -> p c t", p=128), o_st[:], reads=[b_ost])

    def ln_tile(self, es, z, b_z, st, b_st, junk, b_junk, s1col):
        kb = self.kb
        kb.op("act", lambda e: e.activation(out=junk[:], in_=z[:], func=AF.Square, accum_out=st[:, 1:2]),
              reads=[b_z], writes=[b_junk, b_st])
        kb.op("dve", lambda e: e.tensor_scalar_mul(st[:, 2:3], st[:, s1col:s1col + 1], 1.0 / D), reads=[b_st], writes=[b_st])
        kb.op("dve", lambda e: e.tensor_tensor(out=st[:, 3:4], in0=st[:, 2:3], in1=st[:, 2:3], op=ALU.mult), reads=[b_st], writes=[b_st])
        kb.op("dve", lambda e: e.scalar_tensor_tensor(out=st[:, 4:5], in0=st[:, 1:2], scalar=1.0 / D, in1=st[:, 3:4],
                                                      op0=ALU.mult, op1=ALU.subtract), reads=[b_st], writes=[b_st])
        kb.op("act", lambda e: e.activation(out=st[:, 4:5], in_=st[:, 4:5], func=AF.Sqrt, bias=LN_EPS, scale=1.0),
              reads=[b_st], writes=[b_st])
        kb.op("dve", lambda e: e.reciprocal(st[:, 5:6], st[:, 4:5]), reads=[b_st], writes=[b_st])
        kb.op("dve", lambda e: e.scalar_tensor_tensor(out=st[:, 6:7], in0=st[:, 2:3], scalar=-1.0, in1=st[:, 5:6],
                                                      op0=ALU.mult, op1=ALU.mult), reads=[b_st], writes=[b_st])

    def outproj_ln(self, pos_l, li, kj, kind, x_src):
        kb, nc = self.kb, self.nc
        T, NB, NT = self.T, self.NB, self.NT
        wname = ("mla_w_o", "diff_w_o", "sb_w_o")[kind]
        with self.phase() as es:
            wo, b_wo = self.sb(es, "wo", [128, KC, D], BF16)
            wv = self.din[wname][kj].rearrange("(kc p) n -> p kc n", p=128)
            for h in range(8):
                kb.dma("pool", wo[:, h * 2:(h + 1) * 2, :], wv[:, h * 2:(h + 1) * 2, :], writes=[b_wo])
            gt, b_gt = self.sb(es, "gt", [128, D], F32)
            lg, b_lg = self.sb(es, "lg", [128, D], F32)
            lb, b_lb = self.sb(es, "lb", [128, D], F32)
            self.bcast_load("sp", gt[:], b_gt, self.modv[pos_l, 2 * D:3 * D])
            self.bcast_load("sp", lg[:], b_lg, self.din["ln_g"][li, 0])
            self.bcast_load("sp", lb[:], b_lb, self.din["ln_b"][li, 0])
            ot = [self.sb(es, "ot%d" % i, [128, KC, 512], BF16) for i in range(2)]
            xt = [self.sb(es, "xt%d" % i, [128, D], F32) for i in range(2)]
            za = [self.sb(es, "za%d" % i, [128, D], F32) for i in range(2)]
            xo = [self.sb(es, "xo%d" % i, [128, D], F32) for i in range(2)]
            junk, b_junk = self.sb(es, "junk", [128, D], BF16)
            st, b_st = self.sb(es, "st", [128, 8], F32)
            py = [self.ps(es, "py%d" % i, [128, D]) for i in range(2)]
            otv = None if self.pair else self.OT.rearrange("(c p) t -> p c t", p=128)
            for b in range(NB):
                o_t, b_o = ot[b % 2]
                if self.pair:
                    ogv = self.h_loc["OT"].ap().rearrange("(a j p) t -> a j p t", a=2, j=8)
                    for src in range(2):
                        kb.dma("sp", o_t[:, src * 8:(src + 1) * 8, :],
                               ogv[src, :, :, b * 512:(b + 1) * 512].rearrange("j p t -> p j t"), writes=[b_o])
                else:
                    kb.dma("sp", o_t[:], otv[:, :, b * 512:(b + 1) * 512], writes=[b_o])
                for tt in range(4):
                    t = b * 4 + tt
                    x_t, b_x = xt[t % 2]
                    z, b_z = za[t % 2]
                    xo_t, b_xo = xo[t % 2]
                    p_y, b_py = py[t % 2]
                    kb.dma("sp", x_t[:], x_src[t * 128:(t + 1) * 128, :], writes=[b_x])

                    def mm(e, p_y=p_y, o_t=o_t, tt=tt):
                        for n in range(4):
                            for kc in range(KC):
                                r = e.matmul(p_y[:, n * 512:(n + 1) * 512], lhsT=o_t[:, kc, tt * 128:(tt + 1) * 128],
                                             rhs=wo[:, kc, n * 512:(n + 1) * 512], start=(kc == 0), stop=(kc == KC - 1))
                        return r
                    kb.op("pe", mm, reads=[b_o, b_wo], writes=[b_py])
                    kb.op("dve", lambda e, z=z, p_y=p_y: e.tensor_tensor(out=z[:], in0=p_y[:], in1=gt[:], op=ALU.mult),
                          reads=[b_py, b_gt], writes=[b_z])
                    kb.op("dve", lambda e, z=z, x_t=x_t: e.scalar_tensor_tensor(
                        out=z[:], in0=x_t[:], scalar=ALPHA, in1=z[:], op0=ALU.mult, op1=ALU.add, accum_out=st[:, 0:1]),
                        reads=[b_x], writes=[b_z, b_st])
                    self.ln_tile(es, z, b_z, st, b_st, junk, b_junk, 0)
                    kb.op("act", lambda e, z=z: e.activation(out=z[:], in_=z[:], func=AF.Identity,
                                                             bias=st[:, 6:7], scale=st[:, 5:6]),
                          reads=[b_st], writes=[b_z])
                    kb.op("dve", lambda e, z=z: e.tensor_tensor(out=z[:], in0=z[:], in1=lg[:], op=ALU.mult),
                          reads=[b_lg], writes=[b_z])
                    kb.op("dve", lambda e, z=z, xo_t=xo_t: e.tensor_tensor(out=xo_t[:], in0=z[:], in1=lb[:], op=ALU.add),
                          reads=[b_z, b_lb], writes=[b_xo])
                    kb.dma("sp", self.xa[t * 128:(t + 1) * 128, :], xo_t[:], reads=[b_xo])

    def router(self, pos_l, li):
        kb, nc = self.kb, self.nc
        T, NB, NT, C = self.T, self.NB, self.NT, self.C
        with self.phase() as es:
            sc, b_sc = self.sb(es, "sc", [128, D], F32)
            sh, b_sh = self.sb(es, "sh", [128, D], F32)
            self.bcast_load("sp", sc[:], b_sc, self.modv[pos_l, 4 * D:5 * D])
            self.bcast_load("sp", sh[:], b_sh, self.modv[pos_l, 3 * D:4 * D])
            rw, b_rw = self.sb(es, "rw", [128, KC, E], F32)
            kb.dma("sp", rw[:], self.din["moe_router_w"][li].rearrange("(kc p) n -> p kc n", p=128), writes=[b_rw])
            rb, b_rb = self.sb(es, "rb", [1, E], F32)
            kb.dma("sp", rb[:], self.din["moe_router_b"][li:li + 1, :], writes=[b_rb])
            triu, b_triu = self.sb(es, "triu", [128, 128], BF16)
            kb.dma("pool", triu[:], self.din["triu"], writes=[b_triu])
            iotaC, b_iotaC = self.sb(es, "iotaC", [128, E], F32)
            kb.dma("sp", iotaC[:], self.din["iota32"], writes=[b_iotaC])
            kb.op("dve", lambda e: e.tensor_scalar_mul(iotaC[:], iotaC[:], float(C)), reads=[b_iotaC], writes=[b_iotaC])
            cm, b_cm = self.sb(es, "cm", [128, E], BF16)
            kb.op("dve", lambda e: e.memset(cm[:], 0.0), writes=[b_cm])
            xt = [self.sb(es, "xt%d" % i, [128, D], F32) for i in range(2)]
            hb = [self.sb(es, "hb%d" % i, [128, D], BF16) for i in range(2)]
            hT, b_hTr = self.sb(es, "hTr", [128, KC, 128], F32)
            ptr = [self.ps(es, "ptr%d" % i, [128, 4, 128], F32) for i in range(2)]
            plg, b_plg = self.ps(es, "plg", [128, E])
            ppos, b_ppos = self.ps(es, "ppos", [128, E])
            lgs, b_lgs = self.sb(es, "lgs", [128, E], F32)
            m8, b_m8 = self.sb(es, "m8", [128, 8], F32)
            mk, b_mk = self.sb(es, "mk", [128, E], F32)
            mkb, b_mkb = self.sb(es, "mkb", [128, E], BF16)
            ex, b_ex = self.sb(es, "ex", [128, E], F32)
            G, b_G = self.sb(es, "G", [128, E], F32)
            sd, b_sd = self.sb(es, "sd", [128, E], F32)
            ov, b_ov = self.sb(es, "ov", [128, E], F32)
            jk, b_jk = self.sb(es, "jk", [128, E], F32)
            sm, b_sm = self.sb(es, "sm", [128, 8], F32)
            slf, b_slf = self.sb(es, "slf", [128, 4], F32)
            for t in range(NT):
                x_t, b_x = xt[t % 2]
                h_t, b_h = hb[t % 2]
                kb.dma("sp", x_t[:], self.xa[t * 128:(t + 1) * 128, :], writes=[b_x])
                kb.op("dve", lambda e, x_t=x_t: e.tensor_tensor(out=x_t[:], in0=x_t[:], in1=sc[:], op=ALU.mult),
                      reads=[b_sc], writes=[b_x])
                kb.op("dve", lambda e, x_t=x_t: e.tensor_tensor(out=x_t[:], in0=x_t[:], in1=sh[:], op=ALU.add),
                      reads=[b_sh], writes=[b_x])
                kb.op("act", lambda e, x_t=x_t, h_t=h_t: e.copy(h_t[:], x_t[:]), reads=[b_x], writes=[b_h])
                for g in range(4):
                    p_t, b_p = ptr[g % 2]

                    def tr(e, p_t=p_t, x_t=x_t, g=g):
                        for j in range(4):
                            kc = g * 4 + j
                            r = e.transpose(p_t[:, j, :], x_t[:, kc * 128:(kc + 1) * 128], self.ident_f[:])
                        return r
                    kb.op("pe", tr, reads=[b_x, self.b_ident_f], writes=[b_p])
                    kb.op("act" if g % 2 else "dve",
                          (lambda e, p_t=p_t, g=g: e.copy(hT[:, g * 4:(g + 1) * 4, :], p_t[:])) if g % 2 else
                          (lambda e, p_t=p_t, g=g: e.tensor_copy(hT[:, g * 4:(g + 1) * 4, :], p_t[:])),
                          reads=[b_p], writes=[b_hTr])

                def mm(e):
                    for kc in range(KC):
                        e.matmul(plg[:], lhsT=hT[:, kc, :], rhs=rw[:, kc, :], start=(kc == 0), stop=False)
                    return e.matmul(plg[:], lhsT=self.ones_f[0:1, :], rhs=rb[:], start=False, stop=True)
                kb.op("pe", mm, reads=[b_hTr, b_rw, b_rb, self.b_ones_f], writes=[b_plg])
                kb.op("dve", lambda e: e.tensor_copy(lgs[:], plg[:]), reads=[b_plg], writes=[b_lgs])
                kb.op("dve", lambda e: e.max(out=m8[:], in_=lgs[:]), reads=[b_lgs], writes=[b_m8])
                kb.op("dve", lambda e: e.tensor_scalar(out=mk[:], in0=lgs[:], scalar1=m8[:, 3:4], scalar2=None, op0=ALU.is_ge),
                      reads=[b_lgs, b_m8], writes=[b_mk])
                kb.op("dve", lambda e: e.tensor_copy(mkb[:], mk[:]), reads=[b_mk], writes=[b_mkb])
                kb.op("dve", lambda e: e.tensor_scalar_mul(sm[:, 0:1], m8[:, 0:1], -1.0), reads=[b_m8], writes=[b_sm])
                kb.op("act", lambda e: e.activation(out=ex[:], in_=lgs[:], func=AF.Exp, bias=sm[:, 0:1], scale=1.0),
                      reads=[b_lgs, b_sm], writes=[b_ex])
                kb.op("dve", lambda e: e.scalar_tensor_tensor(out=ex[:], in0=ex[:], scalar=1.0, in1=mk[:], op0=ALU.mult,
                                                              op1=ALU.mult, accum_out=sm[:, 1:2]),
                      reads=[b_mk], writes=[b_ex, b_sm])
                kb.op("dve", lambda e: e.reciprocal(sm[:, 2:3], sm[:, 1:2]), reads=[b_sm], writes=[b_sm])
                kb.op("dve", lambda e: e.tensor_scalar(out=G[:], in0=ex[:], scalar1=sm[:, 2:3], scalar2=None, op0=ALU.mult),
                      reads=[b_ex, b_sm], writes=[b_G])

                def mmp(e):
                    e.matmul(ppos[:], lhsT=triu[:], rhs=mkb[:], start=True, stop=False)
                    return e.matmul(ppos[:], lhsT=self.ones[:], rhs=cm[:], start=False, stop=True)
                kb.op("pe", mmp, reads=[b_triu, b_mkb, b_cm, self.b_ones], writes=[b_ppos])
                kb.op("dve", lambda e: e.tensor_scalar(out=ov[:], in0=ppos[:], scalar1=float(C) - 0.5, scalar2=1.0e9,
                                                       op0=ALU.is_ge, op1=ALU.mult), reads=[b_ppos], writes=[b_ov])
                kb.op("dve", lambda e: e.tensor_tensor(out=sd[:], in0=ppos[:], in1=iotaC[:], op=ALU.add),
                      reads=[b_ppos, b_iotaC], writes=[b_sd])
                kb.op("dve", lambda e: e.tensor_tensor(out=sd[:], in0=sd[:], in1=ov[:], op=ALU.add),
                      reads=[b_ov], writes=[b_sd])
                kb.op("dve", lambda e: e.tensor_tensor(out=cm[:], in0=cm[:], in1=mkb[:], op=ALU.add),
                      reads=[b_mkb], writes=[b_cm])
                for k in range(4):
                    kb.op("dve", lambda e, k=k: e.scalar_tensor_tensor(
                        out=jk[:], in0=lgs[:], scalar=m8[:, k:k + 1], in1=sd[:], op0=ALU.is_equal, op1=ALU.mult,
                        accum_out=slf[:, k:k + 1]), reads=[b_lgs, b_m8, b_sd], writes=[b_jk, b_slf])
                    kb.op("dve", lambda e, k=k, t=t: e.scalar_tensor_tensor(
                        out=jk[:], in0=lgs[:], scalar=m8[:, k:k + 1], in1=G[:], op0=ALU.is_equal, op1=ALU.mult,
                        accum_out=self.gates[:, t, k:k + 1]), reads=[b_lgs, b_m8, b_G], writes=[b_jk, self.b_gates])
                kb.op("dve", lambda e, t=t: e.tensor_copy(self.slots[:, t, :], slf[:]), reads=[b_slf], writes=[self.b_slots])
                for k in range(4):
                    kb.dma("pool", self.xs, h_t[:], reads=[b_h, self.b_slots],
                           indirect=dict(out_offset=bass.IndirectOffsetOnAxis(ap=self.slots[:, t, k:k + 1], axis=0),
                                         in_offset=None, bounds_check=self.bc_reg, oob_is_err=False))

    def experts(self, li):
        kb, nc = self.kb, self.nc
        C, NBLK = self.C, self.NBLK
        with self.phase() as es:
            wgu = [self.sb(es, "wgu%d" % i, [128, KC, 512], BF16) for i in range(5)]
            wdn = [self.sb(es, "wdn%d" % i, [128, 6, 1024], BF16) for i in range(4)]
            bgu = [self.sb(es, "bgu%d" % i, [1, 1536], BF16) for i in range(2)]
            bdn = [self.sb(es, "bdn%d" % i, [1, D], BF16) for i in range(2)]
            xsb = [self.sb(es, "xsb%d" % i, [128, D], BF16) for i in range(2)]
            xsT = [self.sb(es, "xsT%d" % i, [128, KC, 128], BF16) for i in range(2)]
            gcl, b_gcl = self.sb(es, "gcl", [128, FE], F32)
            sg, b_sg = self.sb(es, "sg", [128, FE], F32)
            l1, b_l1 = self.sb(es, "l1", [128, FE], F32)
            ab, b_ab = self.sb(es, "ab", [128, FE], BF16)
            aT = [self.sb(es, "aT%d" % i, [128, 6, 128], BF16) for i in range(2)]
            yst = [self.sb(es, "yst%d" % i, [128, D], F32) for i in range(2)]
            p_gu, b_pgu = self.ps(es, "p_gu", [128, 1536])
            p_ys, b_pys = self.ps(es, "p_ys", [128, D])
            p_tr, b_ptr = self.ps(es, "p_tr", [128, 8, 128], BF16)
            gu_v = self.din["moe_w_gate_up"][li]
            dn_v = self.din["moe_w_down"][li]

            def gu_chunk(e, c):
                return wgu[(3 * e + c) % 5]

            def dn_half(e, c):
                return wdn[(e % 2) * 2 + c]

            def load_gu(e, c):
                wv = gu_v[e].rearrange("(kc p) n -> p kc n", p=128)
                w_t, b_w = gu_chunk(e, c)
                for h in range(2):
                    kb.dma("pool", w_t[:, h * 8:(h + 1) * 8, :], wv[:, h * 8:(h + 1) * 8, c * 512:(c + 1) * 512], writes=[b_w])

            def load_dn(e, c):
                dv = dn_v[e].rearrange("(kc p) n -> p kc n", p=128)
                w_t, b_w = dn_half(e, c)
                kb.dma("pool", w_t[:], dv[:, :, c * 1024:(c + 1) * 1024], writes=[b_w])

            def load_bias(e):
                kb.dma("pool", bgu[e % 2][0][:], self.din["moe_b_gate_up"][li, e:e + 1, :], writes=[bgu[e % 2][1]])
                kb.dma("pool", bdn[e % 2][0][:], self.din["moe_b_down"][li, e:e + 1, :], writes=[bdn[e % 2][1]])

            blocks = [(e, blk) for e in range(E) for blk in range(NBLK)]

            def load_xs(i):
                e, blk = blocks[i]
                r0 = e * C + blk * 128
                kb.dma("sp", xsb[i % 2][0][:], self.xs[r0:r0 + 128, :], writes=[xsb[i % 2][1]])

            def TRX(i):
                x_t, b_x = xsb[i % 2]
                xT, b_xT = xsT[i % 2]
                for half in range(2):
                    def tr(e_):
                        for j in range(8):
                            kc = half * 8 + j
                            r = e_.transpose(p_tr[:, j, :], x_t[:, kc * 128:(kc + 1) * 128], self.ident[:])
                        return r
                    kb.op("pe", tr, reads=[b_x, self.b_ident], writes=[b_ptr])
                    kb.op("act", lambda e_: e_.copy(xT[:, half * 8:(half + 1) * 8, :], p_tr[:]),
                          reads=[b_ptr], writes=[b_xT])

            def G(i):
                e, blk = blocks[i]
                xT, b_xT = xsT[i % 2]
                bg, b_bg = bgu[e % 2]
                for c in range(3):
                    w_t, b_w = gu_chunk(e, c)

                    def mm(e_):
                        for kc in range(KC):
                            e_.matmul(p_gu[:, c * 512:(c + 1) * 512], lhsT=xT[:, kc, :], rhs=w_t[:, kc, :],
                                      start=(kc == 0), stop=False)
                        return e_.matmul(p_gu[:, c * 512:(c + 1) * 512], lhsT=self.ones[0:1, :],
                                         rhs=bg[:, c * 512:(c + 1) * 512], start=False, stop=True)
                    kb.op("pe", mm, reads=[b_xT, b_bg, self.b_ones, b_w], writes=[b_pgu])

            def ACTV(i):
                kb.op("dve", lambda e_: e_.tensor_scalar_min(gcl[:], p_gu[:, 0:FE], 7.0), reads=[b_pgu], writes=[b_gcl])
                kb.op("act", lambda e_: e_.activation(out=sg[:], in_=gcl[:], func=AF.Sigmoid, scale=1.702),
                      reads=[b_gcl], writes=[b_sg])
                kb.op("dve", lambda e_: e_.tensor_scalar(out=l1[:], in0=p_gu[:, FE:2 * FE], scalar1=-7.0, scalar2=7.0,
                                                        op0=ALU.max, op1=ALU.min), reads=[b_pgu], writes=[b_l1])
                kb.op("dve", lambda e_: e_.scalar_tensor_tensor(out=l1[:], in0=l1[:], scalar=1.0, in1=gcl[:],
                                                               op0=ALU.add, op1=ALU.mult), reads=[b_gcl], writes=[b_l1])
                kb.op("dve", lambda e_: e_.tensor_tensor(out=ab[:], in0=l1[:], in1=sg[:], op=ALU.mult),
                      reads=[b_l1, b_sg], writes=[b_ab])

            def TA(i):
                a_T, b_aT = aT[i % 2]

                def tr2(e_):
                    for j in range(6):
                        r = e_.transpose(p_tr[:, j, :], ab[:, j * 128:(j + 1) * 128], self.ident[:])
                    return r
                kb.op("pe", tr2, reads=[b_ab, self.b_ident], writes=[b_ptr])
                kb.op("act", lambda e_: e_.copy(a_T[:], p_tr[:, 0:6, :]), reads=[b_ptr], writes=[b_aT])

            def DN(i):
                e, blk = blocks[i]
                a_T, b_aT = aT[i % 2]
                y_t, b_y = yst[i % 2]
                bd, b_bd = bdn[e % 2]
                r0 = e * C + blk * 128
                for hf in range(2):
                    w_t, b_w = dn_half(e, hf)

                    def mm3(e_):
                        for n in range(2 * hf, 2 * hf + 2):
                            cs = slice((n % 2) * 512, (n % 2 + 1) * 512)
                            for f in range(6):
                                e_.matmul(p_ys[:, n * 512:(n + 1) * 512], lhsT=a_T[:, f, :], rhs=w_t[:, f, cs],
                                          start=(f == 0), stop=False)
                            r = e_.matmul(p_ys[:, n * 512:(n + 1) * 512], lhsT=self.ones[0:1, :],
                                          rhs=bd[:, n * 512:(n + 1) * 512], start=False, stop=True)
                        return r
                    kb.op("pe", mm3, reads=[b_aT, b_bd, self.b_ones, b_w], writes=[b_pys])
                kb.op("act", lambda e_: e_.copy(y_t[:], p_ys[:]), reads=[b_pys], writes=[b_y])
                kb.dma("sp", self.ys[r0:r0 + 128, :], y_t[:], reads=[b_y])

            for c in range(3):
                load_gu(0, c)
            load_bias(0)
            load_dn(0, 0)
            load_dn(0, 1)
            load_xs(0)
            TRX(0)
            nb = len(blocks)
            for i, (e, blk) in enumerate(blocks):
                if i + 1 < nb:
                    load_xs(i + 1)
                G(i)
                if blk == NBLK - 1 and e + 1 < E:
                    load_gu(e + 1, 2)
                ACTV(i)
                if i > 0:
                    DN(i - 1)
                if blk == 0 and e + 1 < E:
                    load_gu(e + 1, 0)
                    load_gu(e + 1, 1)
                    load_bias(e + 1)
                    load_dn(e + 1, 0)
                    load_dn(e + 1, 1)
                TA(i)
                if i + 1 < nb:
                    TRX(i + 1)
            DN(nb - 1)

    def combine_ln(self, pos_l, li, x_dst):
        kb, nc = self.kb, self.nc
        T, NT, C = self.T, self.NT, self.C
        with self.phase() as es:
            gt, b_gt = self.sb(es, "gt", [128, D], F32)
            lg, b_lg = self.sb(es, "lg", [128, D], F32)
            lb, b_lb = self.sb(es, "lb", [128, D], F32)
            self.bcast_load("sp", gt[:], b_gt, self.modv[pos_l, 5 * D:6 * D])
            self.bcast_load("sp", lg[:], b_lg, self.din["ln_g"][li, 1])
            self.bcast_load("sp", lb[:], b_lb, self.din["ln_b"][li, 1])
            yg = [self.sb(es, "yg%d" % i, [128, 4, D], F32) for i in range(2)]
            xt = [self.sb(es, "xt%d" % i, [128, D], F32) for i in range(2)]
            za = [self.sb(es, "za%d" % i, [128, D], F32) for i in range(2)]
            xo = [self.sb(es, "xo%d" % i, [128, D], F32) for i in range(2)]
            junk, b_junk = self.sb(es, "junk", [128, D], BF16)
            st, b_st = self.sb(es, "st", [128, 8], F32)
            for t in range(NT):
                y_t, b_y = yg[t % 2]
                x_t, b_x = xt[t % 2]
                z, b_z = za[t % 2]
                xo_t, b_xo = xo[t % 2]
                kb.dma("sp", x_t[:], self.xa[t * 128:(t + 1) * 128, :], writes=[b_x])
                kb.op("pool", lambda e, y_t=y_t: e.memset(y_t[:], 0.0), writes=[b_y])
                for k in range(4):
                    kb.dma("pool", y_t[:, k, :], self.ys, reads=[self.b_slots], writes=[b_y],
                           indirect=dict(out_offset=None,
                                         in_offset=bass.IndirectOffsetOnAxis(ap=self.slots[:, t, k:k + 1], axis=0),
                                         bounds_check=self.bc_reg, oob_is_err=False))
                kb.op("dve", lambda e, z=z, y_t=y_t, t=t: e.tensor_scalar(
                    out=z[:], in0=y_t[:, 0, :], scalar1=self.gates[:, t, 0:1], scalar2=None, op0=ALU.mult),
                    reads=[b_y, self.b_gates], writes=[b_z])
                for k in range(1, 4):
                    kb.op("dve", lambda e, z=z, y_t=y_t, t=t, k=k: e.scalar_tensor_tensor(
                        out=z[:], in0=y_t[:, k, :], scalar=self.gates[:, t, k:k + 1], in1=z[:], op0=ALU.mult, op1=ALU.add),
                        reads=[b_y, self.b_gates], writes=[b_z])
                kb.op("dve", lambda e, z=z: e.tensor_tensor(out=z[:], in0=z[:], in1=gt[:], op=ALU.mult),
                      reads=[b_gt], writes=[b_z])
                kb.op("dve", lambda e, z=z, x_t=x_t: e.scalar_tensor_tensor(
                    out=z[:], in0=x_t[:], scalar=ALPHA, in1=z[:], op0=ALU.mult, op1=ALU.add, accum_out=st[:, 0:1]),
                    reads=[b_x], writes=[b_z, b_st])
                self.ln_tile(es, z, b_z, st, b_st, junk, b_junk, 0)
                kb.op("act", lambda e, z=z: e.activation(out=z[:], in_=z[:], func=AF.Identity,
                                                         bias=st[:, 6:7], scale=st[:, 5:6]), reads=[b_st], writes=[b_z])
                kb.op("dve", lambda e, z=z: e.tensor_tensor(out=z[:], in0=z[:], in1=lg[:], op=ALU.mult),
                      reads=[b_lg], writes=[b_z])
                kb.op("dve", lambda e, z=z, xo_t=xo_t: e.tensor_tensor(out=xo_t[:], in0=z[:], in1=lb[:], op=ALU.add),
                      reads=[b_z, b_lb], writes=[b_xo])
                kb.dma("sp", x_dst[t * 128:(t + 1) * 128, :], xo_t[:], reads=[b_xo])


WEIGHT_NAMES = ["ada_w", "ada_b", "ln_g", "ln_b", "mla_w_in", "mla_q_norm_g", "mla_kv_norm_g", "mla_w_uq",
                "mla_w_ukv", "mla_w_o", "diff_w_qkv", "diff_lambda", "diff_subln_g", "diff_w_o", "sb_w_qkv",
                "sb_w_o", "moe_router_w", "moe_router_b", "moe_w_gate_up", "moe_b_gate_up", "moe_w_down",
                "moe_b_down"]


def run_cores(cfg, core_inputs):
    shapes = {k: v.shape for k, v in core_inputs[0].items()}
    prog = Prog(cfg, shapes)
    nc = prog.build()
    res = run_bass_kernel_spmd(nc, core_inputs, core_ids=list(range(len(core_inputs))))
    return res, prog


def kernel(**inputs):
    x = np.asarray(inputs["x"], dtype=np.float32)
    c = np.asarray(inputs["c"], dtype=np.float32)
    B, S, _ = x.shape
    weights = {k: np.ascontiguousarray(np.asarray(inputs[k], dtype=np.float32)) for k in WEIGHT_NAMES}
    layers = [(i, i % 3, i, i // 3) for i in range(DEPTH)]
    T = S // 2
    cfg = {"T": T, "C": 512, "layers": layers, "pair": True}
    core_inputs = []
    for b in range(B):
        for r in range(2):
            d = dict(weights)
            d.update(make_consts(np.arange(r * T, (r + 1) * T)))
            d["x"] = np.ascontiguousarray(x[b, r * T:(r + 1) * T])
            d["c16"] = np.ascontiguousarray(c[b].reshape(KC, 128).T)
            core_inputs.append(d)
    res, _ = run_cores(cfg, core_inputs)
    out = np.stack([np.concatenate([res.results[2 * b]["y"], res.results[2 * b + 1]["y"]], 0) for b in range(B)], 0)
    return out.astype(np.float32)
```
